# Optimizing a Trainium2 kernel written in Bass

```python
import math
import jax
import jax.numpy as jnp
from jax import lax
import numpy as np

D_MODEL = 1024
BATCH = 4
SEQ = 4096
DEPTH = 1

GRID_W = 64
CTX_LEN = 256
D_MIX = D_MODEL
EPS = 1e-6
CONV_W = 5
NEG_BIG = -1e30

SSD_WIDTH = D_MIX // 2
SSD_HEADDIM = 64
SSD_HEADS = SSD_WIDTH // SSD_HEADDIM
SSD_GROUPS = 2
SSD_STATE = 128
SSD_CHUNK = 128
SSD_BC = SSD_GROUPS * SSD_STATE
SSD_CONV_DIM = SSD_WIDTH + 2 * SSD_BC
SSD_IN = SSD_WIDTH + SSD_CONV_DIM + 2 * SSD_HEADS

ML_WIDTH = D_MIX - SSD_WIDTH
ML_HEADS = 4
ML_HEADDIM = ML_WIDTH // ML_HEADS
ML_QK_BLOCK = 4
ML_CHUNK = 128
ML_IN = 3 * ML_WIDTH + 4 * ML_HEADS

D_IN = SSD_IN + ML_IN

MOE_GROUPS = 4
MOE_EXPERTS_PER_GROUP = 8
MOE_EXPERTS = MOE_GROUPS * MOE_EXPERTS_PER_GROUP
MOE_TOP_K = 2
D_EXPERT = D_MODEL // 4

kernel_name = "hymba_ssd_mlstm_hmoe_dit_block"


def rms_norm(t, w):
    tf = t.astype(jnp.float32)
    y = tf * lax.rsqrt(jnp.mean(tf * tf, axis=-1, keepdims=True) + EPS)
    return (y * w.astype(jnp.float32)).astype(t.dtype)


def modulate(t, shift, scale):
    return t * (1 + scale) + shift


def dwconv_centred(t, w, bias):
    pad = CONV_W // 2
    length = t.shape[1]
    tp = jnp.pad(t, ((0, 0), (pad, pad), (0, 0)))
    out = bias
    for tap in range(CONV_W):
        out = out + w[tap] * tp[:, tap:tap + length]
    return out


def to_col_major(t, rows):
    b, length, ch = t.shape
    return t.reshape(b, rows, GRID_W, ch).transpose(0, 2, 1, 3).reshape(b, length, ch)


def from_col_major(t, rows):
    b, length, ch = t.shape
    return t.reshape(b, GRID_W, rows, ch).transpose(0, 2, 1, 3).reshape(b, length, ch)


def segsum(a):
    size = a.shape[-1]
    cs = jnp.cumsum(a, axis=-1)
    mask = jnp.tril(jnp.ones((size, size), dtype=bool))
    return jnp.where(mask, cs[..., :, None] - cs[..., None, :], -jnp.inf)


def ssd_scan(xs, dt, a_neg, bm, cm, init_state):
    b, length, nh, hp = xs.shape
    nc = length // SSD_CHUNK
    rep = nh // SSD_GROUPS
    bh = jnp.repeat(bm, rep, axis=2)
    ch = jnp.repeat(cm, rep, axis=2)

    def chunk(t):
        return t.reshape((b, nc, SSD_CHUNK) + t.shape[2:])

    xc, bc, cc = chunk(xs * dt[..., None]), chunk(bh), chunk(ch)
    ac = chunk(dt * a_neg).transpose(0, 3, 1, 2)
    a_cs = jnp.cumsum(ac, axis=-1)
    l_mat = jnp.exp(segsum(ac))
    y_diag = jnp.einsum("bclhn,bcshn,bhcls,bcshp->bclhp", cc, bc, l_mat, xc)
    decay_states = jnp.exp(a_cs[..., -1:] - a_cs)
    states = jnp.einsum("bclhn,bhcl,bclhp->bchpn", bc, decay_states, xc)
    states = jnp.concatenate([init_state[:, None], states], axis=1)
    chunk_decay = jnp.exp(segsum(jnp.pad(a_cs[..., -1], ((0, 0), (0, 0), (1, 0)))))
    new_states = jnp.einsum("bhzc,bchpn->bzhpn", chunk_decay, states)
    prev_states, final_state = new_states[:, :-1], new_states[:, -1]
    y_off = jnp.einsum("bclhn,bchpn,bhcl->bclhp", cc, prev_states, jnp.exp(a_cs))
    return (y_diag + y_off).reshape(b, length, nh, hp), final_state


def ssd_group(p_lat, p_ctx, conv_w, conv_b, dt_bias, a_log, d_skip, norm_w):
    a_neg = -jnp.exp(a_log.astype(jnp.float32))

    def prep(p):
        b, length, _ = p.shape
        z = p[..., :SSD_WIDTH]
        xbc = jax.nn.silu(dwconv_centred(p[..., SSD_WIDTH:SSD_WIDTH + SSD_CONV_DIM], conv_w, conv_b)).astype(jnp.float32)
        dt_raw = p[..., SSD_WIDTH + SSD_CONV_DIM:].astype(jnp.float32).reshape(b, length, 2, SSD_HEADS)
        dt = jax.nn.softplus(dt_raw + dt_bias.astype(jnp.float32))
        xs = xbc[..., :SSD_WIDTH].reshape(b, length, SSD_HEADS, SSD_HEADDIM)
        bm = xbc[..., SSD_WIDTH:SSD_WIDTH + SSD_BC].reshape(b, length, SSD_GROUPS, SSD_STATE)
        cm = xbc[..., SSD_WIDTH + SSD_BC:].reshape(b, length, SSD_GROUPS, SSD_STATE)
        return z, xs, bm, cm, dt

    zl, xl, bl, cl, dtl = prep(p_lat)
    zc, xc, bc, cc, dtc = prep(p_ctx)
    s0 = jnp.zeros((xl.shape[0], SSD_HEADS, SSD_HEADDIM, SSD_STATE), jnp.float32)
    yc_f, s_f = ssd_scan(xc, dtc[:, :, 0], a_neg[0], bc, cc, s0)
    yl_f, _ = ssd_scan(xl, dtl[:, :, 0], a_neg[0], bl, cl, s_f)
    yc_b, s_b = ssd_scan(xc[:, ::-1], dtc[:, ::-1, 1], a_neg[1], bc[:, ::-1], cc[:, ::-1], s0)
    yl_b, _ = ssd_scan(xl[:, ::-1], dtl[:, ::-1, 1], a_neg[1], bl[:, ::-1], cl[:, ::-1], s_b)

    def finish(yf, yb, xs, z):
        b, length = xs.shape[:2]
        y = yf + yb[:, ::-1] + d_skip.astype(jnp.float32)[:, None] * xs
        y = y.reshape(b, length, SSD_WIDTH) * jax.nn.silu(z.astype(jnp.float32))
        return rms_norm(y, norm_w)

    return finish(yl_f, yl_b, xl, zl), finish(yc_f, yc_b, xc, zc)


def mlstm_scan(q, k, v, log_i, log_f, state):
    b, nh, length, dh = q.shape
    nc = length // ML_CHUNK

    def chunk(t):
        return t.reshape((b, nh, nc, ML_CHUNK) + t.shape[3:])

    qc, kc, vc = chunk(q), chunk(k), chunk(v)
    ic, fc = chunk(log_i), chunk(log_f)
    bcum = jnp.cumsum(fc, axis=-1)
    b_last = bcum[..., -1]
    a = b_last[..., None] - bcum + ic
    m_loc = jnp.max(a, axis=-1)
    wgt = jnp.exp(a - m_loc[..., None])
    c_loc = jnp.einsum("bhcs,bhcsd,bhcse->bhcde", wgt, vc, kc)
    n_loc = jnp.einsum("bhcs,bhcse->bhce", wgt, kc)

    def step(carry, inp):
        c_st, n_st, m_st = carry
        c_l, n_l, m_l, b_l = inp
        m_new = jnp.maximum(b_l + m_st, m_l)
        s_prev = jnp.exp(b_l + m_st - m_new)
        s_loc = jnp.exp(m_l - m_new)
        c_new = s_prev[..., None, None] * c_st + s_loc[..., None, None] * c_l
        n_new = s_prev[..., None] * n_st + s_loc[..., None] * n_l
        return (c_new, n_new, m_new), (c_st, n_st, m_st)

    final, prev = lax.scan(step, state, (jnp.moveaxis(c_loc, 2, 0), jnp.moveaxis(n_loc, 2, 0), jnp.moveaxis(m_loc, 2, 0), jnp.moveaxis(b_last, 2, 0)))
    c_prev = jnp.moveaxis(prev[0], 0, 2)
    n_prev = jnp.moveaxis(prev[1], 0, 2)
    m_prev = jnp.moveaxis(prev[2], 0, 2)
    mask = jnp.tril(jnp.ones((ML_CHUNK, ML_CHUNK), dtype=bool))
    d_log = jnp.where(mask, bcum[..., :, None] - bcum[..., None, :] + ic[..., None, :], -jnp.inf)
    g_inter = bcum + m_prev[..., None]
    m_out = jnp.maximum(g_inter, jnp.max(d_log, axis=-1))
    scores = jnp.einsum("bhctd,bhcsd->bhcts", qc, kc) * jnp.exp(d_log - m_out[..., None])
    w_inter = jnp.exp(g_inter - m_out)
    num = jnp.einsum("bhcts,bhcsd->bhctd", scores, vc) + w_inter[..., None] * jnp.einsum("bhcde,bhcte->bhctd", c_prev, qc)
    den = jnp.sum(scores, axis=-1) + w_inter * jnp.einsum("bhce,bhcte->bhct", n_prev, qc)
    h = num / jnp.maximum(jnp.abs(den), jnp.exp(-m_out))[..., None]
    return h.reshape(b, nh, length, dh), final


def mlstm_group(p_lat, p_ctx, conv_w, conv_b, w_qk, gate_b, norm_w, skip):
    def prep(p):
        b, length, _ = p.shape
        x_m = p[..., :ML_WIDTH]
        v = p[..., ML_WIDTH:2 * ML_WIDTH]
        o = p[..., 2 * ML_WIDTH:3 * ML_WIDTH]
        g = p[..., 3 * ML_WIDTH:]
        xconv = jax.nn.silu(dwconv_centred(x_m, conv_w, conv_b)).astype(jnp.float32)
        blocks = xconv.reshape(b, length, ML_WIDTH // ML_QK_BLOCK, ML_QK_BLOCK)
        qk = jnp.einsum("blnj,snjk->sblnk", blocks, w_qk.astype(jnp.float32))

        def heads(t):
            return t.reshape(b, length, ML_HEADS, ML_HEADDIM).transpose(0, 2, 1, 3)

        q = heads(qk[0])
        k = heads(qk[1]) * (ML_HEADDIM ** -0.5)
        vh = heads(v.astype(jnp.float32))
        gates = (g.astype(jnp.float32).reshape(b, length, 2, 2, ML_HEADS) + gate_b.astype(jnp.float32)).transpose(2, 3, 0, 4, 1)
        log_i = gates[0]
        log_f = jax.nn.log_sigmoid(gates[1])
        return xconv, o, q, k, vh, log_i, log_f

    xl, ol, ql, kl, vl, il, fl = prep(p_lat)
    xc, oc, qc, kc, vc, ic, fc = prep(p_ctx)
    b = ql.shape[0]
    state0 = (jnp.zeros((b, ML_HEADS, ML_HEADDIM, ML_HEADDIM), jnp.float32), jnp.zeros((b, ML_HEADS, ML_HEADDIM), jnp.float32), jnp.full((b, ML_HEADS), NEG_BIG, jnp.float32))
    hc_f, st_f = mlstm_scan(qc, kc, vc, ic[0], fc[0], state0)
    hl_f, _ = mlstm_scan(ql, kl, vl, il[0], fl[0], st_f)
    hc_b, st_b = mlstm_scan(qc[:, :, ::-1], kc[:, :, ::-1], vc[:, :, ::-1], ic[1][..., ::-1], fc[1][..., ::-1], state0)
    hl_b, _ = mlstm_scan(ql[:, :, ::-1], kl[:, :, ::-1], vl[:, :, ::-1], il[1][..., ::-1], fl[1][..., ::-1], st_b)

    def finish(hf, hb, xconv, o):
        bb, length = xconv.shape[:2]
        h = (hf + hb[:, :, ::-1]).transpose(0, 2, 1, 3)
        h = jax.nn.sigmoid(o.astype(jnp.float32)).reshape(bb, length, ML_HEADS, ML_HEADDIM) * h
        h = h * lax.rsqrt(jnp.mean(h * h, axis=-1, keepdims=True) + EPS)
        return h.reshape(bb, length, ML_WIDTH) * norm_w.astype(jnp.float32) + skip.astype(jnp.float32) * xconv

    return finish(hl_f, hl_b, xl, ol), finish(hc_f, hc_b, xc, oc)


def hmoe(t_in, rg_w, rg_b, re_w, re_b, w_gate, w_up, w_down):
    b, length, d = t_in.shape
    t = t_in.reshape(b * length, d)
    g_logits = (t @ rg_w + rg_b).astype(jnp.float32)
    g_prob = jax.nn.softmax(g_logits, axis=-1)
    g_sel = jnp.argmax(g_logits, axis=-1)
    p_group = jnp.take_along_axis(g_prob, g_sel[:, None], axis=-1)
    e_logits = (t @ re_w + re_b).astype(jnp.float32).reshape(-1, MOE_GROUPS, MOE_EXPERTS_PER_GROUP)
    e_logits = jnp.take_along_axis(e_logits, g_sel[:, None, None], axis=1)[:, 0]
    top_logit, top_idx = lax.top_k(e_logits, MOE_TOP_K)
    top_w = jax.nn.softmax(top_logit, axis=-1) * p_group
    w_in_group = jnp.einsum("tk,tke->te", top_w, jax.nn.one_hot(top_idx, MOE_EXPERTS_PER_GROUP, dtype=jnp.float32))
    combine = jax.nn.one_hot(g_sel, MOE_GROUPS, dtype=jnp.float32)[:, :, None] * w_in_group[:, None, :]
    y = jnp.zeros_like(t)
    for grp in range(MOE_GROUPS):
        lo = grp * MOE_EXPERTS_PER_GROUP
        hi = lo + MOE_EXPERTS_PER_GROUP
        hidden = jax.nn.silu(jnp.einsum("td,edf->tef", t, w_gate[lo:hi])) * jnp.einsum("td,edf->tef", t, w_up[lo:hi])
        hidden = hidden * combine[:, grp, :, None].astype(hidden.dtype)
        y = y + jnp.einsum("tef,efd->td", hidden, w_down[lo:hi])
    return y.reshape(b, length, d)


def setup_inputs(seed: int = 0) -> dict:
    key = jax.random.key(seed)
    ks = iter(jax.random.split(key, 40))
    f32 = jnp.float32

    def nrm(shape, scale):
        return scale * jax.random.normal(next(ks), shape, f32)

    def gain(shape):
        return 1.0 + 0.02 * jax.random.normal(next(ks), shape, f32)

    nl = DEPTH
    x = nrm((BATCH, SEQ, D_MODEL), 1.0)
    c = nrm((BATCH, D_MODEL), 1.0)
    ctx = nrm((BATCH, CTX_LEN, D_MODEL), 1.0)
    c_ctx = nrm((D_MODEL,), 1.0)
    w_mod = nrm((nl, D_MODEL, 6 * D_MODEL), 0.5 * D_MODEL ** -0.5)
    b_mod = nrm((nl, 6 * D_MODEL), 0.01)
    norm1_w = gain((nl, D_MODEL))
    w_in = nrm((nl, D_MODEL, D_IN), D_MODEL ** -0.5)
    ssd_conv_w = nrm((nl, CONV_W, SSD_CONV_DIM), CONV_W ** -0.5)
    ssd_conv_b = nrm((nl, SSD_CONV_DIM), 0.01)
    dt0 = jnp.exp(jax.random.uniform(next(ks), (nl, 2, SSD_HEADS), f32, math.log(1e-3), math.log(1e-1)))
    ssd_dt_bias = dt0 + jnp.log(-jnp.expm1(-dt0))
    ssd_a_log = jnp.log(jax.random.uniform(next(ks), (nl, 2, SSD_HEADS), f32, 1.0, 16.0))
    ssd_d = gain((nl, SSD_HEADS))
    ssd_norm_w = gain((nl, SSD_WIDTH))
    ml_conv_w = nrm((nl, CONV_W, ML_WIDTH), CONV_W ** -0.5)
    ml_conv_b = nrm((nl, ML_WIDTH), 0.01)
    ml_w_qk = nrm((nl, 2, ML_WIDTH // ML_QK_BLOCK, ML_QK_BLOCK, ML_QK_BLOCK), ML_QK_BLOCK ** -0.5)
    ml_gate_b = jnp.stack([nrm((nl, 2, ML_HEADS), 0.1), 3.0 + nrm((nl, 2, ML_HEADS), 0.5)], axis=1)
    ml_norm_w = gain((nl, ML_WIDTH))
    ml_skip = gain((nl, ML_WIDTH))
    w_out = nrm((nl, D_MIX, D_MODEL), D_MIX ** -0.5)
    norm2_w = gain((nl, D_MODEL))
    moe_rg_w = nrm((nl, D_MODEL, MOE_GROUPS), D_MODEL ** -0.5)
    moe_rg_b = nrm((nl, MOE_GROUPS), 0.01)
    moe_re_w = nrm((nl, D_MODEL, MOE_EXPERTS), D_MODEL ** -0.5)
    moe_re_b = nrm((nl, MOE_EXPERTS), 0.01)
    moe_w_gate = nrm((nl, MOE_EXPERTS, D_MODEL, D_EXPERT), D_MODEL ** -0.5)
    moe_w_up = nrm((nl, MOE_EXPERTS, D_MODEL, D_EXPERT), D_MODEL ** -0.5)
    moe_w_down = nrm((nl, MOE_EXPERTS, D_EXPERT, D_MODEL), D_EXPERT ** -0.5)
    final_norm_w = gain((D_MODEL,))
    return {"x": x, "c": c, "ctx": ctx, "c_ctx": c_ctx, "w_mod": w_mod, "b_mod": b_mod, "norm1_w": norm1_w, "w_in": w_in, "ssd_conv_w": ssd_conv_w, "ssd_conv_b": ssd_conv_b, "ssd_dt_bias": ssd_dt_bias, "ssd_a_log": ssd_a_log, "ssd_d": ssd_d, "ssd_norm_w": ssd_norm_w, "ml_conv_w": ml_conv_w, "ml_conv_b": ml_conv_b, "ml_w_qk": ml_w_qk, "ml_gate_b": ml_gate_b, "ml_norm_w": ml_norm_w, "ml_skip": ml_skip, "w_out": w_out, "norm2_w": norm2_w, "moe_rg_w": moe_rg_w, "moe_rg_b": moe_rg_b, "moe_re_w": moe_re_w, "moe_re_b": moe_re_b, "moe_w_gate": moe_w_gate, "moe_w_up": moe_w_up, "moe_w_down": moe_w_down, "final_norm_w": final_norm_w}


def reference(x, c, ctx, c_ctx, w_mod, b_mod, norm1_w, w_in, ssd_conv_w, ssd_conv_b, ssd_dt_bias, ssd_a_log, ssd_d, ssd_norm_w, ml_conv_w, ml_conv_b, ml_w_qk, ml_gate_b, ml_norm_w, ml_skip, w_out, norm2_w, moe_rg_w, moe_rg_b, moe_re_w, moe_re_b, moe_w_gate, moe_w_up, moe_w_down, final_norm_w):
    rows = x.shape[1] // GRID_W
    for layer in range(DEPTH):
        mod_lat = (jax.nn.silu(c) @ w_mod[layer] + b_mod[layer])[:, None, :]
        mod_ctx = jax.nn.silu(c_ctx) @ w_mod[layer] + b_mod[layer]
        sh1, sc1, g1, sh2, sc2, g2 = jnp.split(mod_lat, 6, axis=-1)
        csh1, csc1, cg1, csh2, csc2, cg2 = jnp.split(mod_ctx, 6, axis=-1)
        h_lat = modulate(rms_norm(x, norm1_w[layer]), sh1, sc1)
        h_ctx = modulate(rms_norm(ctx, norm1_w[layer]), csh1, csc1)
        p_lat = h_lat @ w_in[layer]
        p_ctx = h_ctx @ w_in[layer]
        y_ssd_lat, y_ssd_ctx = ssd_group(p_lat[..., :SSD_IN], p_ctx[..., :SSD_IN], ssd_conv_w[layer], ssd_conv_b[layer], ssd_dt_bias[layer], ssd_a_log[layer], ssd_d[layer], ssd_norm_w[layer])
        y_ml_lat, y_ml_ctx = mlstm_group(to_col_major(p_lat[..., SSD_IN:], rows), p_ctx[..., SSD_IN:], ml_conv_w[layer], ml_conv_b[layer], ml_w_qk[layer], ml_gate_b[layer], ml_norm_w[layer], ml_skip[layer])
        y_lat = jnp.concatenate([y_ssd_lat, from_col_major(y_ml_lat, rows)], axis=-1).astype(x.dtype)
        x = x + g1 * (y_lat @ w_out[layer])
        x = x + g2 * hmoe(modulate(rms_norm(x, norm2_w[layer]), sh2, sc2), moe_rg_w[layer], moe_rg_b[layer], moe_re_w[layer], moe_re_b[layer], moe_w_gate[layer], moe_w_up[layer], moe_w_down[layer])
        if layer < DEPTH - 1:
            y_ctx = jnp.concatenate([y_ssd_ctx, y_ml_ctx], axis=-1).astype(ctx.dtype)
            ctx = ctx + cg1 * (y_ctx @ w_out[layer])
            ctx = ctx + cg2 * hmoe(modulate(rms_norm(ctx, norm2_w[layer]), csh2, csc2), moe_rg_w[layer], moe_rg_b[layer], moe_re_w[layer], moe_re_b[layer], moe_w_gate[layer], moe_w_up[layer], moe_w_down[layer])
    return rms_norm(x, final_norm_w)
```

```python
import numpy as np
from contextlib import ExitStack
import concourse.bass as bass
import concourse.mybir as mybir
from concourse.bass_utils import run_bass_kernel_spmd

F32 = mybir.dt.float32
BF16 = mybir.dt.bfloat16
AF = mybir.ActivationFunctionType
ALU = mybir.AluOpType

D = 1024
T = 4096
TC = 256
TOWN = 2048
EPS = 1e-6
DEBUG = set()


class _Op:
    __slots__ = ("eng", "meth", "args", "kw", "reads", "writes", "dma", "deps", "odeps", "signal", "sem", "val", "prewait", "idx")


class Prog:
    ENGS = ("pe", "act", "dve", "pool", "sp")

    def __init__(self, nc, n_dma_sems=12):
        self.nc = nc
        self.ops = []
        self.n_dma_sems = n_dma_sems
        self.fence_res = None
        self.all_res = set()

    def I(self, eng, meth, args, kw=None, r=(), w=(), dma=False):
        o = _Op()
        o.eng = eng; o.meth = meth; o.args = args; o.kw = kw or {}
        r = tuple(r); w = tuple(w)
        pb_ = tuple(x for x in r if len(x) == 2 and x[0] == "b" and x[1].isdigit())
        if pb_:
            r = tuple(x for x in r if x not in pb_)
            w = w + tuple(x for x in pb_ if x not in w)
        o.reads = r + ((self.fence_res,) if self.fence_res else ())
        o.writes = w
        o.dma = dma; o.deps = []; o.signal = dma; o.sem = None; o.val = 0; o.prewait = None
        o.idx = len(self.ops)
        self.ops.append(o)
        self.all_res.update(o.reads); self.all_res.update(o.writes)
        return o

    def fence(self):
        name = "__fence%d" % len(self.ops)
        res = [x for x in self.all_res if not x.startswith("__fence")]
        self.I("pool", "nop", (), {}, r=(), w=res + [name])
        self.fence_res = name

    def mm(self, out, lhsT, rhs, start, stop, r, w):
        return self.I("pe", "matmul", (out, lhsT, rhs), dict(start=start, stop=stop), r, w)

    def dma(self, out, in_, r, w, q="sp"):
        return self.I(q, "dma_start", (), dict(out=out, in_=in_), r, w, dma=True)

    def act(self, out, in_, func, r, w, **kw):
        return self.I("act", "activation", (out, in_, func), kw, r, w)

    def _dur(self, o):
        def free(ap):
            n = 1
            for x in ap.shape[1:]:
                n *= x
            return n
        try:
            if o.dma:
                ap = o.kw["out"]
                return 2.0 + free(ap) * ap.shape[0] * (4 if ap.dtype == F32 else 2) / 150e3
            if o.meth == "matmul":
                out, lhsT = o.args[0], o.args[1]
                return 0.03 + max(free(out), 32) / 2000.0 * (4 if lhsT.dtype == F32 else 1)
            if o.meth == "nop":
                return 0.05
            ap = o.args[0]
            n = free(ap)
            if o.meth == "reciprocal":
                return 0.15 + n / 160.0
            if o.eng == "act":
                return 0.25 + n / 1200.0
            if o.eng == "pool":
                return 0.3 + n / 500.0
            return 0.15 + n / 960.0
        except Exception:
            return 0.5

    def finalize(self, stack):
        nc = self.nc
        ops = self.ops
        last_w = {}
        readers = {}
        for o in ops:
            deps = {}
            odeps = {}

            def add(d, raw):
                if d is None or d is o:
                    return
                odeps[d.idx] = d
                same = (d.eng == o.eng) and not d.dma and not o.dma
                if same and not raw and o.eng == "pe":
                    return
                deps[d.idx] = d

            for x in o.reads:
                add(last_w.get(x), True)
            for x in o.writes:
                add(last_w.get(x), False)
                for rd in readers.get(x, ()):
                    add(rd, False)
            o.deps = list(deps.values())
            o.odeps = list(odeps.values())
            for d in o.deps:
                d.signal = True
            for x in o.reads:
                readers.setdefault(x, []).append(o)
            for x in o.writes:
                last_w[x] = o
                readers[x] = []
        per0 = {e: [o for o in ops if o.eng == e] for e in self.ENGS}
        W = 400
        fin_t = {}
        eng_free = {e: 0.0 for e in self.ENGS}
        pending = {e: list(per0[e]) for e in self.ENGS}
        order = {e: [] for e in self.ENGS}
        durs = {o.idx: self._dur(o) for o in ops}
        remaining = len(ops)
        while remaining:
            best = None
            for e in self.ENGS:
                lst = pending[e]
                lim = min(W, len(lst))
                for j in range(lim):
                    o = lst[j]
                    t = eng_free[e]
                    ok = True
                    for d in o.odeps:
                        ft = fin_t.get(d.idx)
                        if ft is None:
                            ok = False
                            break
                        if ft > t:
                            t = ft
                    if not ok:
                        continue
                    key = (t, o.idx)
                    if best is None or key < best[0]:
                        best = (key, e, j, o, t)
                    if t <= eng_free[e]:
                        break
            assert best is not None, "scheduler stuck"
            _, e, j, o, t = best
            pending[e].pop(j)
            order[e].append(o)
            if o.dma:
                eng_free[e] = t + 0.15
                fin_t[o.idx] = t + durs[o.idx]
            else:
                eng_free[e] = t + durs[o.idx]
                fin_t[o.idx] = t + durs[o.idx] + 0.1
            remaining -= 1
        self.sim_time = max(fin_t.values()) if fin_t else 0.0
        pos = {}
        for e in self.ENGS:
            for i_, o in enumerate(order[e]):
                pos[o.idx] = i_
        for o in ops:
            if not o.dma:
                o.signal = False
        for o in ops:
            bye = {}
            keep = []
            for d in o.deps:
                if d.dma:
                    keep.append(d)
                else:
                    c_ = bye.get(d.eng)
                    if c_ is None or pos[d.idx] > pos[c_.idx]:
                        bye[d.eng] = d
            keep.extend(bye.values())
            o.deps = keep
            for d in keep:
                d.signal = True
        esem = {}
        for e in ("pe", "act", "dve", "pool"):
            esem[e] = stack.enter_context(nc.semaphore("s_" + e))
        dsems = {}
        for q in ("sp", "pool", "act"):
            if any(o.dma and o.eng == q for o in ops):
                dsems[q] = [stack.enter_context(nc.semaphore("d_%s_%d" % (q, i))) for i in range(self.n_dma_sems)]
        cnt = {e: 0 for e in esem}
        dcnt = {q: [0] * self.n_dma_sems for q in dsems}
        drr = {q: 0 for q in dsems}
        for e in self.ENGS:
            for o in order[e]:
                if o.dma:
                    i = drr[o.eng]
                    drr[o.eng] = (i + 1) % self.n_dma_sems
                    o.sem = dsems[o.eng][i]
                    if dcnt[o.eng][i] > 0:
                        o.prewait = (o.sem, dcnt[o.eng][i])
                    dcnt[o.eng][i] += 16
                    o.val = dcnt[o.eng][i]
                elif o.signal:
                    cnt[o.eng] += 1
                    o.sem = esem[o.eng]
                    o.val = cnt[o.eng]
        self.sem_counts = dict(cnt)
        per = order
        block = stack.enter_context(nc.Block())

        def emit(engname):
            lst = per[engname]

            def body(e):
                known = {}
                for o in lst:
                    waits = [(d.sem, d.val) for d in o.deps]
                    if o.prewait is not None:
                        waits.append(o.prewait)
                    for (s, v) in waits:
                        k = id(s)
                        if known.get(k, 0) >= v:
                            continue
                        e.wait_ge(s, v)
                        known[k] = v
                    ins = getattr(e, o.meth)(*o.args, **o.kw)
                    if o.dma:
                        ins.then_inc(o.sem, 16)
                    elif o.signal:
                        ins.then_inc(o.sem, 1)
                if engname in dsems:
                    for i, s in enumerate(dsems[engname]):
                        if dcnt[engname][i] > 0:
                            e.wait_ge(s, dcnt[engname][i])

            return body

        if per["pe"]:
            block.tensor(emit("pe"))
        if per["act"]:
            block.scalar(emit("act"))
        if per["dve"]:
            block.vector(emit("dve"))
        if per["pool"]:
            block.gpsimd(emit("pool"))
        if per["sp"]:
            block.sync(emit("sp"))


class Arena:
    def __init__(self, ap, nwords):
        self.ap = ap
        self.n = nwords
        self.off = 0
        self.cnt = 0

    def alloc(self, shape, dt=F32, name=None):
        free = 1
        for s in shape[1:]:
            free *= s
        words = free if dt == F32 else (free + 1) // 2
        words += words & 1
        assert self.off + words <= self.n, "arena overflow %d + %d > %d" % (self.off, words, self.n)
        v = self.ap[0:shape[0], self.off:self.off + words]
        self.off += words
        if dt != F32:
            v = v.bitcast(dt)[:, 0:free]
        if len(shape) > 2:
            names = ["a%d" % i for i in range(len(shape) - 1)]
            pat = "p (%s) -> p %s" % (" ".join(names), " ".join(names))
            v = v.rearrange(pat, **{n: s for n, s in zip(names[:-1], shape[1:-1])})
        self.cnt += 1
        return v, (name or "t") + "#%d" % self.cnt

    def mark(self):
        return self.off

    def release(self, m):
        self.off = m


def bc(ap, axis, n):
    v = ap.unsqueeze(axis)
    shp = list(v.shape)
    shp[axis] = n
    return v.to_broadcast(shp)


def build_nc():
    nc = bass.Bass("TRN2", target_bir_lowering=False)

    def din(name, shape, dt=F32):
        return nc.dram_tensor(name, list(shape), dt, kind="ExternalInput").ap()

    def scr(name, shape, dt=F32):
        kind = "ExternalOutput" if name in DEBUG else "Internal"
        return nc.dram_tensor(name, list(shape), dt, kind=kind).ap()

    xT = {"ssd": din("xT_r", [T // 256, 128, 8 * 256]), "ml": din("xT_c", [T // 256, 128, 8 * 256])}
    ctxT = din("ctxT", [1, 128, 8 * 256])
    x_own = din("x_own", [TOWN, D])
    ccol = din("ccol", [128, 8, 2])
    w_mod = din("w_mod", [D, 6 * D])
    bmod_col = din("bmod_col", [128, 16])
    bmod_row = din("bmod_row", [1, 6 * D])
    n1w_col = din("n1w_col", [128, 8])
    w_in = din("w_in", [D, 3104])
    ssd_cw = din("ssd_cw", [128, 8, 5])
    ssd_cb = din("ssd_cb", [128, 8])
    ssd_row = din("ssd_row", [1, 16 + 16 + 8 + 512])
    ml_cw = din("ml_cw", [128, 4, 5])
    ml_cb = din("ml_cb", [128, 4])
    wqk = din("wqk", [2, 4, 128, 128])
    ml_row = din("ml_row", [1, 16 + 512 + 512])
    w_out = din("w_out", [D, D])
    n2w_row = din("n2w_row", [1, D])
    w_r = din("w_r", [D, 36])
    rb_row = din("rb_row", [1, 36])
    wg = din("wg", [32, D, 256])
    wu = din("wu", [32, D, 256])
    wd = din("wd", [32, 256, D])
    fnw_row = din("fnw_row", [1, D])
    cst = din("cst", [128, 6, 128])
    out = nc.dram_tensor("out", [TOWN, D], F32, kind="ExternalOutput").ap()

    convT = {("ssd", "lat"): scr("cv_ssd_lat", [8, 128, T], BF16), ("ssd", "ctx"): scr("cv_ssd_ctx", [8, 128, TC], BF16),
             ("ml", "lat"): scr("cv_ml_lat", [4, 128, T], BF16), ("ml", "ctx"): scr("cv_ml_ctx", [4, 128, TC], BF16)}
    z_tm = scr("z_tm", [TOWN, 512], BF16)
    v_tm = {"lat": scr("v_lat", [T, 512], BF16), "ctx": scr("v_ctx", [TC, 512], BF16)}
    o_tm = scr("o_tm", [T, 512], BF16)
    yf_ssd = scr("yf_ssd", [TOWN, 512], F32)
    hf_ml = scr("hf_ml", [T, 512], F32)
    ymix = scr("ymix", [TOWN, D], BF16)
    x1s = scr("x1s", [TOWN, D], F32)
    modrow = scr("modrow", [1, 4 * D], F32)

    st = ExitStack()
    NW = 53000
    arena_t = st.enter_context(nc.sbuf_tensor("arena", [128, NW], F32))
    A = Arena(arena_t, NW)
    PS = st.enter_context(nc.psum_tensor("ps", [128, 8, 512], F32))
    P = Prog(nc)

    def bank(i, n=1):
        if n == 1:
            return PS[:, i, :], ["b%d" % i]
        return PS[:, i:i + n, :], ["b%d" % j for j in range(i, i + n)]

    def V(eng, meth, *args, r=(), w=(), **kw):
        return P.I(eng, meth, args, kw, r, w)

    C32, rC32 = A.alloc([128, 6, 128], F32, "C32")
    CB, rCB = A.alloc([128, 6, 128], BF16, "CB")
    P.dma(C32, cst, [], [rC32])
    V("dve", "tensor_copy", CB, C32, r=[rC32], w=[rCB])
    identb, Ub, onesb, MNb_ = CB[:, 0, :], (CB[:, 1, :], CB[:, 2, :]), CB[:, 3, :], (CB[:, 4, :], CB[:, 5, :])
    U32 = (C32[:, 1, :], C32[:, 2, :])
    ones32 = C32[:, 3, :]
    negones, rnegones = A.alloc([128, 128], BF16, "negones")
    V("dve", "tensor_scalar", negones, onesb, -1.0, None, ALU.mult, r=[rCB], w=[rnegones])
    MNrep = []
    for d_ in range(2):
        t_, r_ = A.alloc([128, 4, 128], BF16, "MNrep")
        V("dve", "tensor_copy", t_, bc(MNb_[d_], 1, 4), r=[rCB], w=[r_])
        MNrep.append((t_, r_))

    def bcrow(src_ap, n, name, q="sp"):
        t_, r_ = A.alloc([128, n], F32, name)
        P.dma(t_, src_ap.partition_broadcast(128), [], [r_], q=q)
        return t_, r_

    ssdrow, rssdrow = bcrow(ssd_row, 552, "ssdrow")
    mlrow, rmlrow = bcrow(ml_row, 1040, "mlrow")
    dtb_bc = ssdrow[:, 0:16]
    aneg_bc, raneg = A.alloc([128, 16], F32, "aneg")
    P.act(aneg_bc, ssdrow[:, 16:32], AF.Exp, [rssdrow], [raneg])
    V("dve", "tensor_scalar", aneg_bc, aneg_bc, -1.0, None, ALU.mult, r=[raneg], w=[raneg])
    Dh_bc = ssdrow[:, 32:40]
    ssdnw_bc = ssdrow[:, 40:552]
    gb_bc = mlrow[:, 0:16]
    mlnw_bc = mlrow[:, 16:528]
    mlskip_bc = mlrow[:, 528:1040]

    dt_all = {}; a_all = {}; gi_all = {}; af_all = {}
    for tag, nt in (("lat", 32), ("ctx", 2)):
        dt_all[tag] = A.alloc([128, nt, 16], F32, "dt_" + tag)
        a_all[tag] = A.alloc([128, nt, 16], F32, "a_" + tag)
        gi_all[tag] = A.alloc([128, nt, 8], F32, "gi_" + tag)
        af_all[tag] = A.alloc([128, nt, 8], F32, "af_" + tag)

    cols, rcols = A.alloc([128, 4, 8], F32, "cols")
    m0 = A.mark()
    cc, rcc = A.alloc([128, 8, 2], F32, "cc")
    sil, rsil = A.alloc([128, 8, 2], F32, "sil")
    P.dma(cc, ccol, [], [rcc])
    P.act(sil, cc, AF.Silu, [rcc], [rsil])
    silT, rsilT = A.alloc([128, 8, 64], F32, "silT")
    V("dve", "memset", silT, 0.0, r=[], w=[rsilT])
    V("dve", "tensor_copy", silT[:, :, 0:1], sil[:, :, 0:1], r=[rsil, rsilT], w=[rsilT])
    V("dve", "tensor_copy", silT[:, :, 32:33], sil[:, :, 1:2], r=[rsil, rsilT], w=[rsilT])
    bmc, rbmc = A.alloc([128, 16], F32, "bmc")
    n1c, rn1c = A.alloc([128, 8], F32, "n1c")
    P.dma(bmc, bmod_col, [], [rbmc])
    P.dma(n1c, n1w_col, [], [rn1c])
    brow, rbrow = A.alloc([1, 4 * D], F32, "brow")
    P.dma(brow, bmod_row[:, 2 * D:6 * D], [], [rbrow])
    rowm, rrowm = A.alloc([64, 6 * D], F32, "rowm")
    wst = [A.alloc([128, 8, 512], F32, "wst%d" % i) for i in range(4)]
    wmod_v = w_mod.rearrange("(k p) n -> p k n", p=128)
    for nb_ in range(12):
        ws, rws = wst[nb_ % 4]
        P.dma(ws, wmod_v[:, :, nb_ * 512:(nb_ + 1) * 512], [], [rws], q=("sp" if nb_ % 2 == 0 else "act"))
        pr, rpr = bank(nb_ % 2)
        for k in range(8):
            P.mm(pr[0:64, :], silT[:, k, :], ws[:, k, :], k == 0, k == 7, [rws, rsilT], rpr)
        rr_ = rrowm + "_%d" % nb_
        if nb_ % 2 == 0:
            V("act", "copy", rowm[:, nb_ * 512:(nb_ + 1) * 512], pr[0:64, :], r=rpr, w=[rr_])
        else:
            V("dve", "tensor_copy", rowm[:, nb_ * 512:(nb_ + 1) * 512], pr[0:64, :], r=rpr, w=[rr_])
    pc, rpc = bank(2)
    for src_ in range(2):
        pp_ = 32 * src_
        for vec in range(2):
            for j in range(8):
                idx = (src_ * 2 + vec) * 8 + j
                c0_ = vec * D + j * 128
                P.mm(pc[:, idx:idx + 1], rowm[pp_:pp_ + 1, c0_:c0_ + 128], C32[pp_:pp_ + 1, 3, 0:1], True, True,
                     [rrowm + "_%d" % (c0_ // 512), rC32], rpc)
    modc, rmodc = A.alloc([128, 2, 2, 8], F32, "modc")
    V("dve", "tensor_copy", modc, pc[:, 0:32].rearrange("p (s v j) -> p s v j", s=2, v=2), r=rpc, w=[rmodc])
    for j in range(2):
        V("dve", "tensor_tensor", cols[:, 2 * j + 1, :], modc[:, j, 0, :], bmc[:, 0:8], ALU.add, r=[rmodc, rbmc], w=[rcols])
        V("dve", "tensor_tensor", cols[:, 2 * j, :], modc[:, j, 1, :], bmc[:, 8:16], ALU.add, r=[rmodc, rbmc], w=[rcols])
        V("dve", "scalar_tensor_tensor", cols[:, 2 * j, :], cols[:, 2 * j, :], 1.0, n1c, ALU.add, ALU.mult, r=[rcols, rn1c], w=[rcols])
    V("dve", "tensor_tensor", rowm[0:1, 2 * D:6 * D], rowm[0:1, 2 * D:6 * D], brow, ALU.add,
      r=[rrowm + "_%d" % i for i in range(4, 12)] + [rbrow], w=[rrowm + "_g"])
    P.dma(modrow, rowm[0:1, 2 * D:6 * D], [rrowm + "_g"], ["modrow"], q="sp")
    P.fence()
    A.release(m0)

    win_v = w_in.rearrange("(k p) n -> p k n", p=128)

    def inproj_pass(mx):
        m1 = A.mark()
        col0 = 0 if mx == "ssd" else 1552
        nch = 8 if mx == "ssd" else 4
        cvc0 = 512 if mx == "ssd" else 0
        Wb, rWb = A.alloc([128, 8, 1552], BF16, "Wb")
        stgW = [A.alloc([128, 8, 512], F32, "stg%d" % i) for i in range(2)]
        stg = []
        for (t_, r_) in stgW:
            stg.append((t_[:, :, 0:256], r_ + "a"))
            stg.append((t_[:, :, 256:512], r_ + "b"))
        for blk in range(4):
            s_, rs_ = stgW[blk % 2]
            rsW = [rs_ + "a", rs_ + "b"]
            sv = s_[:, :, 0:388]
            P.dma(sv, win_v[:, :, col0 + blk * 388: col0 + (blk + 1) * 388], [], rsW, q=("sp" if blk % 2 == 0 else "pool"))
            if blk % 2 == 0:
                V("dve", "tensor_copy", Wb[:, :, blk * 388:(blk + 1) * 388], sv, r=rsW, w=[rWb])
            else:
                V("act", "copy", Wb[:, :, blk * 388:(blk + 1) * 388], sv, r=rsW, w=[rWb])
        cw_t, rcw = A.alloc([128, nch, 5], F32, "cw")
        cb_t, rcb = A.alloc([128, nch], F32, "cb")
        P.dma(cw_t, ssd_cw if mx == "ssd" else ml_cw, [], [rcw])
        P.dma(cb_t, ssd_cb if mx == "ssd" else ml_cb, [], [rcb])
        DG, rDG = A.alloc([128, nch, 5, 128], BF16, "DG")
        for j in range(nch):
            for tap in range(5):
                V("dve", "tensor_scalar", DG[:, j, tap, :], identb, cw_t[:, j, tap:tap + 1], None, ALU.mult, r=[rCB, rcw], w=[rDG + "_%d_%d" % (j, tap)])
        hT_l, rhT_l = A.alloc([128, 8, T], BF16, "hT")
        hT_c, rhT_c = A.alloc([128, 8, TC], BF16, "hTc")
        sq2 = [A.alloc([128, 8, 256], BF16, "sq%d" % i) for i in range(2)]
        rsb2 = [A.alloc([128, 256], F32, "rsb%d" % i) for i in range(2)]
        pre_l = [A.alloc([128, T + 4], BF16, "pre%d" % i) for i in range(2)]
        pre_c = [A.alloc([128, TC + 4], BF16, "prec%d" % i) for i in range(2)]
        cv_l = [A.alloc([128, T], BF16, "cv%d" % i) for i in range(2)]
        cv_c = [A.alloc([128, TC], BF16, "cvc%d" % i) for i in range(2)]
        ev = [A.alloc([128, 512], BF16, "ev%d" % i) for i in range(4)]
        sm4 = [A.alloc([128, 16], F32, "sm%d" % i) for i in range(4)]
        for (p_, rp_) in pre_l + pre_c:
            V("pool", "memset", p_, 0.0, r=[], w=[rp_])
        evi = [0]
        smi = [0]
        bbi = [0]
        sbi = [0]

        def big_bank():
            bbi[0] += 1
            return 6 + bbi[0] % 2

        def small_bank():
            sbi[0] += 1
            return sbi[0] % 2
        for tag in ("ctx", "lat"):
            hT, rhT = (hT_c, rhT_c) if tag == "ctx" else (hT_l, rhT_l)
            pre = pre_c if tag == "ctx" else pre_l
            cvs = cv_c if tag == "ctx" else cv_l
            Tn = TC if tag == "ctx" else T
            TB = 256
            src = ctxT if tag == "ctx" else xT[mx]
            scol = cols[:, 2, :] if tag == "ctx" else cols[:, 0, :]
            shcol = cols[:, 3, :] if tag == "ctx" else cols[:, 1, :]
            for tb in range(Tn // TB):
                s_, rs_ = stg[tb % 4]
                sq, rsq = sq2[tb % 2]
                rsb, rrsb = rsb2[tb % 2]
                xs = s_[:, :, 0:TB]
                P.dma(xs, src[tb].rearrange("p (k t) -> p k t", k=8), [], [rs_], q=("sp" if tb % 2 == 0 else "pool"))
                P.act(sq[:, :, 0:TB], xs, AF.Square, [rs_], [rsq])
                pb, rpb = bank(tb % 2)
                for k in range(8):
                    P.mm(pb[:, 0:TB], onesb, sq[:, k, 0:TB], k == 0, k == 7, [rCB, rsq], rpb)
                P.act(rsb[:, 0:TB], pb[:, 0:TB], AF.Ln, rpb, [rrsb], scale=1.0 / D, bias=EPS)
                P.act(rsb[:, 0:TB], rsb[:, 0:TB], AF.Exp, [rrsb], [rrsb], scale=-0.5)
                V("dve", "tensor_tensor", xs, xs, bc(rsb[:, 0:TB], 1, 8), ALU.mult, r=[rs_, rrsb], w=[rs_])
                for k in range(8):
                    hdst = hT[:, k, tb * TB:(tb + 1) * TB]
                    if k in (0, 1, 2):
                        P.act(hdst, xs[:, k, :], AF.Identity, [rs_, rcols], [rhT], scale=scol[:, k:k + 1], bias=shcol[:, k:k + 1])
                    else:
                        V("dve", "tensor_scalar", hdst, xs[:, k, :], scol[:, k:k + 1], shcol[:, k:k + 1], ALU.mult, ALU.add, r=[rs_, rcols], w=[rhT])
        for tag in ("ctx", "lat"):
            hT, rhT = (hT_c, rhT_c) if tag == "ctx" else (hT_l, rhT_l)
            pre = pre_c if tag == "ctx" else pre_l
            cvs = cv_c if tag == "ctx" else cv_l
            Tn = TC if tag == "ctx" else T
            TB = 256 if tag == "ctx" else 512
            xdep = [rhT_l] if tag == "ctx" else []
            cvd = convT[(mx, tag)]
            for j in range(nch):
                p_, rp_ = pre[j % 2]
                cv, rcv = cvs[j % 2]
                wc = cvc0 + j * 128
                for tb in range(Tn // TB):
                    pb, rpb = bank(2 + tb % 2)
                    for k in range(8):
                        P.mm(pb[:, 0:TB], Wb[:, k, wc:wc + 128], hT[:, k, tb * TB:(tb + 1) * TB], k == 0, k == 7, [rWb, rhT] + xdep, rpb)
                    if tb % 2 == 0:
                        V("act", "copy", p_[:, 2 + tb * TB:2 + (tb + 1) * TB], pb[:, 0:TB], r=rpb, w=[rp_])
                    else:
                        V("dve", "tensor_copy", p_[:, 2 + tb * TB:2 + (tb + 1) * TB], pb[:, 0:TB], r=rpb, w=[rp_])
                for tb in range(Tn // TB):
                    pb, rpb = bank(4 + tb % 2)
                    for tap in range(5):
                        P.mm(pb[:, 0:TB], DG[:, j, tap, :], p_[:, tb * TB + tap: tb * TB + tap + TB], tap == 0, tap == 4, [rDG + "_%d_%d" % (j, tap), rp_], rpb)
                    P.act(cv[:, tb * TB:(tb + 1) * TB], pb[:, 0:TB], AF.Silu, rpb + [rcb], [rcv], bias=cb_t[:, j:j + 1])
                P.dma(cvd[j], cv[:, 0:Tn], [rcv], ["cvd_%s_%s" % (mx, tag)], q=("sp" if j % 2 == 0 else "pool"))
            for tt in range(Tn // 128):
                tsl = slice(tt * 128, (tt + 1) * 128)

                def tm(colA, ncols, bk):
                    pb, rpb = bank(bk)
                    for k in range(8):
                        P.mm(pb[:, 0:ncols], hT[:, k, tsl], Wb[:, k, colA:colA + ncols], k == 0, k == 7, [rhT, rWb] + xdep, rpb)
                    return pb[:, 0:ncols], rpb

                def evac_store(pv, rpv, dst):
                    e_, re_ = ev[evi[0] % 4]
                    if evi[0] % 2 == 0:
                        V("act", "copy", e_, pv, r=rpv, w=[re_])
                    else:
                        V("dve", "tensor_copy", e_, pv, r=rpv, w=[re_])
                    P.dma(dst, e_, [re_], ["dst_" + dst.name if hasattr(dst, "name") else "dst"], q=("sp" if evi[0] % 2 == 0 else "pool"))
                    evi[0] += 1

                sm, rsm = sm4[smi[0] % 4]
                smi[0] += 1
                if mx == "ssd":
                    if tag == "lat" and tt < 16:
                        pv, rpv = tm(0, 512, big_bank())
                        evac_store(pv, rpv, z_tm[tsl, :])
                    pv, rpv = tm(1536, 16, small_bank())
                    dt_, rdt = dt_all[tag]
                    a_, ra = a_all[tag]
                    V("dve", "tensor_tensor", sm, pv, dtb_bc, ALU.add, r=rpv + [rssdrow], w=[rsm])
                    P.act(sm, sm, AF.Exp, [rsm], [rsm])
                    P.act(dt_[:, tt, :], sm, AF.Ln, [rsm], [rdt], bias=1.0)
                    V("dve", "tensor_tensor", a_[:, tt, :], dt_[:, tt, :], aneg_bc, ALU.mult, r=[rdt, raneg], w=[ra])
                else:
                    pv, rpv = tm(512, 512, big_bank())
                    evac_store(pv, rpv, v_tm[tag][tsl, :])
                    if tag == "lat":
                        pv, rpv = tm(1024, 512, big_bank())
                        evac_store(pv, rpv, o_tm[tsl, :])
                    pv, rpv = tm(1536, 16, small_bank())
                    gi_, rgi = gi_all[tag]
                    af_, raf = af_all[tag]
                    V("dve", "tensor_tensor", sm, pv, gb_bc, ALU.add, r=rpv + [rmlrow], w=[rsm])
                    V("dve", "tensor_copy", gi_[:, tt, :], sm[:, 0:8], r=[rsm], w=[rgi])
                    P.act(sm[:, 8:16], sm[:, 8:16], AF.Exp, [rsm], [rsm], scale=-1.0)
                    P.act(sm[:, 8:16], sm[:, 8:16], AF.Ln, [rsm], [rsm], bias=1.0)
                    V("dve", "tensor_scalar", af_[:, tt, :], sm[:, 8:16], -1.0, None, ALU.mult, r=[rsm], w=[raf])
        P.fence()
        A.release(m1)

    def scan_mixer(mx):
        m1 = A.mark()
        ssd = mx == "ssd"
        H = 8 if ssd else 4
        G = 2 if ssd else 4
        hg = H // G
        dv = 64 if ssd else 130
        slot = 64 if ssd else 256
        nb = 1 if ssd else 2
        nR = 2 if ssd else 1
        dva = dv + (dv & 1)
        def al3(name, dt_):
            t_, r_ = A.alloc([128, H, dva], dt_, name)
            return t_[:, :, 0:dv], r_
        S32 = [al3("S32_%d" % d_, F32) for d_ in range(2)]
        Sbf = [al3("Sbf_%d" % d_, BF16) for d_ in range(2)]
        XB = [A.alloc([128, 8 if ssd else 4, 128], BF16, "XB%d" % i) for i in range(3)]
        if ssd:
            xs_tm2 = [A.alloc([128, 8, 64], BF16, "xs_tm%d" % i) for i in range(3)]
            Vt_2 = [A.alloc([128, 8, 64], BF16, "Vt%d" % i) for i in range(3)]
        else:
            Vt2 = [al3("Vaug%d" % i, BF16) for i in range(3)]
            for (v_, rv_) in Vt2:
                V("pool", "memset", v_, 1.0, r=[], w=[rv_])
            Wqk32, rW32 = A.alloc([128, 2, 4, 128], F32, "Wqk32")
            P.dma(Wqk32, wqk.rearrange("s h p n -> p s h n"), [], [rW32])
            WQ, rWQ = A.alloc([128, 4, 128], BF16, "WQ")
            WKI, rWKI = A.alloc([128, 4, 256], BF16, "WKI")
            V("dve", "tensor_copy", WQ, Wqk32[:, 0, :, :], r=[rW32], w=[rWQ])
            V("dve", "tensor_scalar", WKI[:, :, 0:128], Wqk32[:, 1, :, :], 128.0 ** -0.5, None, ALU.mult, r=[rW32], w=[rWKI])
            V("dve", "tensor_copy", WKI[:, :, 128:256], bc(identb, 1, 4), r=[rCB], w=[rWKI])
            QT2 = [A.alloc([128, 4, 128], BF16, "QT%d" % i) for i in range(3)]
            KT2 = [A.alloc([128, 4, 128], BF16, "KT%d" % i) for i in range(3)]
            xc_tm2 = [A.alloc([128, 4, 128], BF16, "xc_tm%d" % i) for i in range(3)]
            o_t2 = [A.alloc([128, 512], BF16, "o_t%d" % i) for i in range(3)]
        def dbl(shape, dt_, name):
            return [A.alloc(shape, dt_, name + "_%d" % i) for i in range(3)]

        def dbl3(name, dt_):
            return [al3(name + "_%d" % i, dt_) for i in range(3)]

        K_tm2 = dbl([128, G, 128], BF16, "K_tm")
        cs2 = dbl([128, 16], F32, "cs")
        AU2 = dbl([128, H, 128], BF16, "AU")
        Lp2 = dbl([128, H, 128], BF16, "Lp")
        GTs2 = dbl([128, G, 128], BF16, "GTs")
        MT2 = dbl([128, H, 128], BF16, "MT")
        ea2 = dbl([128, 3, 8], F32, "ea")
        VW2 = dbl3("VW", BF16)
        yt2 = dbl3("yt", F32)
        yprev2 = dbl([128, 512], F32, "yprev")
        yo2 = dbl([128, 512], F32, "yo")
        yb162 = dbl([128, 512], BF16, "yb16")
        zt2 = dbl([128, 512], BF16, "zt")
        zs2 = dbl([128, 512], F32, "zs")
        st12 = dbl([128, 8], F32, "st1")
        junk2_ = dbl([128, 512], F32, "junk")

        seqn = [0]

        def step(tag, c, d_, full, upd, fin):
            tsl = slice(c * 128, (c + 1) * 128)
            pp = seqn[0] % 3
            seqn[0] += 1
            xb, rxb = XB[pp]
            K_tm, rKtm = K_tm2[pp]; cs, rcs = cs2[pp]; AU, rAU = AU2[pp]; Lp, rLp = Lp2[pp]
            GTs, rGTs = GTs2[pp]; MT, rMT = MT2[pp]; ea, rea = ea2[pp]; VW, rVW = VW2[pp]; yt, ryt = yt2[pp]
            yprev, ryp = yprev2[pp]; yo, ryo = yo2[pp]; yb16, ryb = yb162[pp]; zt, rzt = zt2[pp]; zs, rzs = zs2[pp]
            st1, rst1 = st12[pp]; junk, rjunk = junk2_[pp]
            if ssd:
                xs_tm, rxs = xs_tm2[pp]; Vt, rV = Vt_2[pp]
            else:
                QT, rQT = QT2[pp]; KT, rKT = KT2[pp]; xc_tm, rxc = xc_tm2[pp]; o_t, ro = o_t2[pp]
            cvd = convT[(mx, tag)]
            P.dma(xb, cvd.rearrange("j p t -> p j t")[:, :, tsl], ["cvd_%s_%s" % (mx, tag)], [rxb], q="sp")
            S32_, rS32 = S32[d_]
            Sb_, rSb = Sbf[d_]
            if full and fin:
                if ssd:
                    P.dma(yprev, yf_ssd[tsl, :], ["yf_ssd"], [ryp], q="pool")
                    P.dma(zt, z_tm[tsl, :], ["dst"], [rzt], q="pool")
                else:
                    P.dma(yprev, hf_ml[tsl, :], ["hf_ml"], [ryp], q="pool")
                    P.dma(o_t, o_tm[tsl, :], ["dst"], [ro], q="pool")
            if ssd:
                a_col = a_all[tag][0][:, c, d_ * 8:(d_ + 1) * 8]
                ra_col = a_all[tag][1]
                pb, rpb = bank(5)
                for j in range(4):
                    P.mm(pb[:, j * 128:(j + 1) * 128], xb[:, j, :], identb, True, True, [rxb, rCB], rpb)
                dtv = dt_all[tag][0][:, c, d_ * 8:(d_ + 1) * 8]
                V("dve", "tensor_tensor", Vt, pb.rearrange("p (h e) -> p h e", h=8), bc(dtv, 2, 64), ALU.mult, r=rpb + [dt_all[tag][1]], w=[rV])
                if fin:
                    V("act", "copy", xs_tm, pb.rearrange("p (h e) -> p h e", h=8), r=rpb, w=[rxs])
                pb7, rpb7 = bank(7)
                for g in range(2):
                    P.mm(pb7[:, g * 128:(g + 1) * 128], xb[:, 4 + g, :], identb, True, True, [rxb, rCB], rpb7)
                V("act", "copy", K_tm, pb7[:, 0:256].rearrange("p (g n) -> p g n", g=2), r=rpb7, w=[rKtm])
                qT = lambda g: xb[:, 6 + g, :]
                kT = lambda g: xb[:, 4 + g, :]
                rq = [rxb]
                Vv, rVv = Vt, rV
                gi = None
                rgi = None
                p0, rp0 = PS[:, 3, 256:272], ["b3"]
            else:
                a_col = af_all[tag][0][:, c, d_ * 4:(d_ + 1) * 4]
                ra_col = af_all[tag][1]
                gi = gi_all[tag][0][:, c, d_ * 4:(d_ + 1) * 4]
                rgi = gi_all[tag][1]
                Vv, rVv = Vt2[pp]
                P.dma(Vv[:, :, 0:128], v_tm[tag][tsl, :].rearrange("t (h e) -> t h e", h=4), ["dst"], [rVv], q="pool")
                pq, rpq = bank(1)
                pk, rpk = bank(2)
                for h in range(4):
                    P.mm(pq[:, h * 128:(h + 1) * 128], WQ[:, h, :], xb[:, h, :], True, True, [rWQ, rxb], rpq)
                for h in range(4):
                    P.mm(pk[:, h * 128:(h + 1) * 128], WKI[:, h, 0:128], xb[:, h, :], True, True, [rWKI, rxb], rpk)
                V("act", "copy", QT, pq.rearrange("p (h n) -> p h n", h=4), r=rpq, w=[rQT])
                V("dve", "tensor_copy", KT, pk.rearrange("p (h n) -> p h n", h=4), r=rpk, w=[rKT])
                pkx, rpkx = bank(1, 2)
                for h in range(4):
                    P.mm(pkx[:, h // 2, (h % 2) * 256:(h % 2) * 256 + 256], xb[:, h, :], WKI[:, h, :], True, True, [rxb, rWKI], rpkx)
                pkx4 = pkx.rearrange("p b (h n) -> p (b h) n", h=2)
                V("act", "copy", K_tm, pkx4[:, :, 0:128], r=rpkx, w=[rKtm])
                if fin:
                    for b_ in range(2):
                        V("act", "copy", xc_tm[:, 2 * b_:2 * b_ + 2, :], pkx[:, b_, :].rearrange("p (h n) -> p h n", h=2)[:, :, 128:256], r=rpkx, w=[rxc])
                qT = lambda g: QT[:, g, :]
                kT = lambda g: KT[:, g, :]
                rq = [rQT, rKT]
                p0, rp0 = PS[:, 0, 0:16], ["b0"]
            P.mm(p0[:, 0:H], U32[d_], a_col, True, True, [rC32, ra_col], rp0)
            P.mm(p0[:, 8:8 + H], ones32, a_col, True, True, [rC32, ra_col], rp0)
            if ssd:
                V("act", "copy", cs, p0[:, 0:16], r=rp0, w=[rcs])
            else:
                V("dve", "tensor_copy", cs, p0[:, 0:16], r=rp0, w=[rcs])
            if full:
                V("dve", "tensor_tensor", AU, bc(Ub[d_], 1, H), bc(a_col, 2, 128), ALU.mult, r=[rCB, ra_col], w=[rAU])
            if upd:
                V("dve", "tensor_tensor", ea[:, 2, 0:H], cs[:, 8:8 + H], cs[:, 0:H], ALU.subtract, r=[rcs], w=[rea])
                if not ssd:
                    V("dve", "tensor_tensor", ea[:, 2, 0:H], ea[:, 2, 0:H], gi, ALU.add, r=[rea, rgi], w=[rea])
            yield
            if full:
                pR, rpR = bank(1, 2)
                for hb in range(nR):
                    hs = slice(hb * 4, hb * 4 + 4)
                    P.mm(pR[:, hb, :], onesb, AU[:, hs, :].rearrange("p h n -> p (h n)"), True, False, [rCB, rAU], rpR)
                    for hh in range(hb * 4, hb * 4 + 4):
                        P.mm(pR[:, hb, (hh % 4) * 128:(hh % 4) * 128 + 128], AU[:, hh, :], negones, False, False, [rAU, rnegones], rpR)
                    P.mm(pR[:, hb, :], identb, MNrep[d_][0].rearrange("p h n -> p (h n)"), False, True, [rCB, MNrep[d_][1]], rpR)
                if ssd:
                    for hb in range(2):
                        P.act(Lp[:, hb * 4:hb * 4 + 4, :], pR[:, hb, :].rearrange("p (h n) -> p h n", h=4), AF.Exp, rpR, [rLp])
                else:
                    for h in range(4):
                        P.act(Lp[:, h, :], pR[:, 0, h * 128:(h + 1) * 128], AF.Exp, rpR + [rgi], [rLp], bias=gi[:, h:h + 1])
                pG, rpG = bank(3)
                for g in range(G):
                    P.mm(pG[:, g * 128:(g + 1) * 128], kT(g), qT(g), True, True, rq, rpG)
                V("act", "copy", GTs, pG[:, 0:G * 128].rearrange("p (g n) -> p g n", g=G), r=rpG, w=[rGTs])
                if ssd:
                    V("dve", "tensor_tensor", MT.rearrange("p (g j) n -> p g j n", g=2), Lp.rearrange("p (g j) n -> p g j n", g=2), bc(GTs, 2, 4), ALU.mult, r=[rLp, rGTs], w=[rMT])
                else:
                    V("dve", "tensor_tensor", MT, Lp, GTs, ALU.mult, r=[rLp, rGTs], w=[rMT])
                P.act(ea[:, 0, 0:H], cs[:, 0:H], AF.Exp, [rcs], [rea])
            if upd:
                P.act(ea[:, 2, 0:H], ea[:, 2, 0:H], AF.Exp, [rea], [rea])
                P.act(ea[:, 1, 0:H], cs[:, 8:8 + H], AF.Exp, [rcs], [rea])
                V("pool", "tensor_tensor", VW, Vv, bc(ea[:, 2, 0:H], 2, dv), ALU.mult, r=[rVv, rea], w=[rVW])
            yield
            if full:
                pY, rpY = bank(4, nb)
                pO, rpO = bank(6, nb)

                def hv(pt, h):
                    if ssd:
                        return pt[:, h * 64:(h + 1) * 64]
                    return pt[:, h // 2, (h % 2) * 256:(h % 2) * 256 + dv]

                for h in range(H):
                    P.mm(hv(pY, h), MT[:, h, :], Vv[:, h, :], True, True, [rMT, rVv], rpY)
                if ssd:
                    for g in range(2):
                        P.mm(pO[:, g * 256:(g + 1) * 256], qT(g), Sb_[:, g * 4:(g + 1) * 4, :].rearrange("p h e -> p (h e)"), True, True, rq + [rSb], rpO)
                else:
                    for h in range(4):
                        P.mm(hv(pO, h), qT(h), Sb_[:, h, :], True, True, rq + [rSb], rpO)
                if ssd:
                    pY3 = pY.rearrange("p (h e) -> p h e", h=8)
                    pO3 = pO.rearrange("p (h e) -> p h e", h=8)
                else:
                    pY3 = pY.rearrange("p b (h n) -> p (b h) n", h=2)[:, :, 0:dv]
                    pO3 = pO.rearrange("p b (h n) -> p (b h) n", h=2)[:, :, 0:dv]
                V("dve", "tensor_tensor", yt, pO3, bc(ea[:, 0, 0:H], 2, dv), ALU.mult, r=rpO + [rea], w=[ryt])
                V("dve", "tensor_tensor", yt, yt, pY3, ALU.add, r=rpY + [ryt], w=[ryt])
            if upd:
                if ssd:
                    pC, rpC = bank(0)
                    for g in range(2):
                        P.mm(pC[:, g * 256:(g + 1) * 256], K_tm[:, g, :], VW[:, g * 4:(g + 1) * 4, :].rearrange("p h e -> p (h e)"), True, True, [rKtm, rVW], rpC)
                    pC3 = pC.rearrange("p (h e) -> p h e", h=8)
                else:
                    pC, rpC = bank(4, 2)
                    for h in range(4):
                        P.mm(pC[:, h // 2, (h % 2) * 256:(h % 2) * 256 + dv], K_tm[:, h, :], VW[:, h, :], True, True, [rKtm, rVW], rpC)
                    pC3 = pC.rearrange("p b (h n) -> p (b h) n", h=2)[:, :, 0:dv]
                V("dve", "tensor_tensor", S32_, S32_, bc(ea[:, 1, 0:H], 2, dv), ALU.mult, r=[rS32, rea], w=[rS32])
                V("dve", "tensor_tensor", S32_, S32_, pC3, ALU.add, r=[rS32] + rpC, w=[rS32])
                V("act", "copy", Sb_, S32_, r=[rS32], w=[rSb])
            if full:
                if ssd:
                    yflat = yt.rearrange("p h e -> p (h e)")
                    if not fin:
                        P.dma(yf_ssd[tsl, :], yflat, [ryt], ["yf_ssd"], q="pool")
                    else:
                        V("dve", "tensor_tensor", yo, yflat, yprev, ALU.add, r=[ryt, ryp], w=[ryo])
                        V("pool", "tensor_tensor", yprev.rearrange("p (h e) -> p h e", h=8), xs_tm, bc(Dh_bc, 2, 64), ALU.mult, r=[rxs, rssdrow], w=[ryp])
                        V("dve", "tensor_tensor", yo, yo, yprev, ALU.add, r=[ryo, ryp], w=[ryo])
                        P.act(zs, zt, AF.Silu, [rzt], [rzs])
                        V("dve", "tensor_tensor", yo, yo, zs, ALU.mult, r=[ryo, rzs], w=[ryo])
                        P.act(junk, yo, AF.Square, [ryo], [rjunk, rst1], accum_out=st1[:, 0:1])
                        P.act(st1[:, 1:2], st1[:, 0:1], AF.Sqrt, [rst1], [rst1], scale=1.0 / 512, bias=EPS)
                        V("dve", "reciprocal", st1[:, 2:3], st1[:, 1:2], r=[rst1], w=[rst1])
                        V("dve", "scalar_tensor_tensor", yb16, yo, st1[:, 2:3], ssdnw_bc, ALU.mult, ALU.mult, r=[ryo, rst1, rssdrow], w=[ryb])
                        P.dma(ymix[tsl, 0:512], yb16, [ryb], ["ymix"], q="sp")
                else:
                    P.act(st1[:, 0:4], yt[:, :, 128], AF.Abs, [ryt], [rst1])
                    V("dve", "tensor_scalar", st1[:, 0:4], st1[:, 0:4], 1.0, None, ALU.max, r=[rst1], w=[rst1])
                    V("dve", "reciprocal", st1[:, 4:8], st1[:, 0:4], r=[rst1], w=[rst1])
                    yo3 = yo.rearrange("p (h e) -> p h e", h=4)
                    V("dve", "tensor_tensor", yo3, yt[:, :, 0:128], bc(st1[:, 4:8], 2, 128), ALU.mult, r=[ryt, rst1], w=[ryo])
                    if not fin:
                        P.dma(hf_ml[tsl, :], yo, [ryo], ["hf_ml"], q="pool")
                    else:
                        V("dve", "tensor_tensor", yo, yo, yprev, ALU.add, r=[ryo, ryp], w=[ryo])
                        P.act(zs, o_t, AF.Tanh, [ro], [rzs], scale=0.5)
                        V("dve", "scalar_tensor_tensor", yo, zs, 1.0, yo, ALU.add, ALU.mult, r=[ryo, rzs], w=[ryo])
                        for h in range(4):
                            P.act(junk[:, h * 128:(h + 1) * 128], yo[:, h * 128:(h + 1) * 128], AF.Square, [ryo], [rjunk, rst1], accum_out=st1[:, h:h + 1])
                        P.act(st1[:, 0:4], st1[:, 0:4], AF.Sqrt, [rst1], [rst1], scale=1.0 / 128, bias=4.0 * EPS)
                        V("dve", "reciprocal", st1[:, 4:8], st1[:, 0:4], r=[rst1], w=[rst1])
                        V("dve", "tensor_tensor", yo3, yo3, bc(st1[:, 4:8], 2, 128), ALU.mult, r=[ryo, rst1], w=[ryo])
                        V("dve", "tensor_tensor", yo, yo, mlnw_bc, ALU.mult, r=[ryo, rmlrow], w=[ryo])
                        V("pool", "tensor_tensor", yprev, xc_tm.rearrange("p h e -> p (h e)"), mlskip_bc, ALU.mult, r=[rxc, rmlrow], w=[ryp])
                        V("dve", "tensor_tensor", yb16, yo, yprev, ALU.add, r=[ryo, ryp], w=[ryb])
                        ymv = ymix.rearrange("(r c) d -> r c d", c=64)
                        P.dma(ymv[:, 2 * c, 512:1024], yb16[0:32, :], [ryb], ["ymix"], q="sp")
                        P.dma(ymv[:, 2 * c + 1, 512:1024], yb16[64:96, :], [ryb], ["ymix"], q="sp")

        def run_pipelined(specs):
            st_a1 = None
            st_a2 = None
            for sp_ in list(specs) + [None, None]:
                g = None
                if sp_ is not None:
                    g = step(*sp_)
                    next(g)
                if st_a1 is not None:
                    next(st_a1)
                if st_a2 is not None:
                    for _ in st_a2:
                        pass
                st_a2 = st_a1
                st_a1 = g

        for d_ in range(2):
            V("pool", "memset", S32[d_][0], 0.0, r=[], w=[S32[d_][1]])
            V("pool", "memset", Sbf[d_][0], 0.0, r=[], w=[Sbf[d_][1]])
        h1 = [("ctx", 0, 0, False, True, False), ("ctx", 1, 1, False, True, False),
              ("ctx", 1, 0, False, True, False), ("ctx", 0, 1, False, True, False)]
        if ssd:
            for i in range(16):
                h1.append(("lat", i, 0, True, i < 15, False))
                h1.append(("lat", 31 - i, 1, False, True, False))
            h2 = [("lat", c, 1, True, c > 0, True) for c in range(15, -1, -1)]
        else:
            for i in range(16):
                h1.append(("lat", i, 0, True, True, False))
                h1.append(("lat", 31 - i, 1, True, True, False))
            h2 = []
            for i in range(16, 32):
                h2.append(("lat", i, 0, True, i < 31, True))
                h2.append(("lat", 31 - i, 1, True, 31 - i > 0, True))
        run_pipelined(h1)
        run_pipelined(h2)
        P.fence()
        A.release(m1)

    def mod_rows():
        g = {}
        modr, rmodr = A.alloc([128, 4, 1024], F32, "modr")
        fnw, rfnw = bcrow(fnw_row, 1024, "fnw", q="pool")
        mD = A.mark()
        for v in range(4):
            P.dma(modr[:, v, :], modrow[:, v * D:(v + 1) * D].partition_broadcast(128), ["modrow"], [rmodr], q=("sp" if v % 2 == 0 else "act"))
        g1_bc, sh2_bc, g2_bc = modr[:, 0, :], modr[:, 1, :], modr[:, 3, :]
        n2w, rn2w = bcrow(n2w_row, 1024, "n2w")
        s2_bc = modr[:, 2, :]
        V("dve", "scalar_tensor_tensor", s2_bc, s2_bc, 1.0, n2w, ALU.add, ALU.mult, r=[rmodr, rn2w], w=[rmodr])

        return dict(modr=modr, rmodr=rmodr, fnw=fnw, rfnw=rfnw, mD=mD, g1_bc=g1_bc, sh2_bc=sh2_bc, g2_bc=g2_bc, s2_bc=s2_bc)

    import os
    STOP = int(os.environ.get("KSTOP", "99"))
    if STOP >= 1:
        inproj_pass("ssd")
    if STOP >= 2:
        scan_mixer("ssd")
    if STOP >= 3:
        inproj_pass("ml")
    MR = mod_rows()
    if STOP >= 4:
        scan_mixer("ml")
    if STOP < 5:
        P.finalize(st)
        st.close()
        print("sem counts", P.sem_counts, "nops", len(P.ops), "sim_us", P.sim_time)
        return nc

    modr, rmodr, fnw, rfnw = MR["modr"], MR["rmodr"], MR["fnw"], MR["rfnw"]
    g1_bc, sh2_bc, g2_bc, s2_bc = MR["g1_bc"], MR["sh2_bc"], MR["g2_bc"], MR["s2_bc"]
    A.release(MR["mD"])
    tT, rtT = A.alloc([128, 8, TOWN], BF16, "tT")
    comb, rcomb = A.alloc([128, 16, 32], F32, "comb")
    xt, rxt = A.alloc([128, 1024], F32, "xt")
    x1, rx1 = A.alloc([128, 1024], F32, "x1")
    junk2, rjunk2 = A.alloc([128, 1024], F32, "junk2")
    s8, rs8 = A.alloc([128, 8], F32, "s8")
    mD = A.mark()
    mstg = [A.alloc([128, 8, 512], F32, "mstgD%d" % i) for i in range(2)]
    rbb, rrbb = bcrow(rb_row, 36, "rbb")
    Wo, rWo = A.alloc([128, 8, 1024], BF16, "Wo")
    wout_v = w_out.rearrange("(k p) n -> p k n", p=128)
    for hb in range(2):
        ms, rms = mstg[hb]
        P.dma(ms, wout_v[:, :, hb * 512:(hb + 1) * 512], [], [rms], q=("sp" if hb == 0 else "pool"))
        V("dve" if hb == 0 else "pool", "tensor_tensor", Wo[:, :, hb * 512:(hb + 1) * 512], ms,
          bc(g1_bc[:, hb * 512:(hb + 1) * 512], 1, 8), ALU.mult, r=[rms, rmodr], w=[rWo])
    Wr32, rWr32 = A.alloc([128, 8, 36], F32, "Wr32")
    Wr, rWr = A.alloc([128, 8, 36], BF16, "Wr")
    P.dma(Wr32, w_r.rearrange("(k p) n -> p k n", p=128), [], [rWr32])
    V("dve", "tensor_copy", Wr, Wr32, r=[rWr32], w=[rWr])
    ym2 = [A.alloc([128, 1024], BF16, "ym%d" % i) for i in range(2)]
    yT2 = [A.alloc([128, 8, 128], BF16, "yT%d" % i) for i in range(2)]
    tb162 = [A.alloc([128, 1024], BF16, "tb16%d" % i) for i in range(2)]
    lg_all, rlg_all = A.alloc([128, 16, 36], F32, "lg_all")
    xtD = [A.alloc([128, 1024], F32, "xtD%d" % i) for i in range(2)]
    x1D = [A.alloc([128, 1024], F32, "x1D%d" % i) for i in range(2)]
    jkD = [A.alloc([128, 1024], F32, "jkD%d" % i) for i in range(2)]
    s8D = [A.alloc([128, 8], F32, "s8D%d" % i) for i in range(2)]
    xt_f, rxt_f, x1_f, rx1_f, junk2_f, rjunk2_f, s8_f, rs8_f = xt, rxt, x1, rx1, junk2, rjunk2, s8, rs8
    for i in range(16):
        tsl = slice(i * 128, (i + 1) * 128)
        ym, rym = ym2[i % 2]; yT, ryT = yT2[i % 2]; tb16, rtb16 = tb162[i % 2]
        xt, rxt = xtD[i % 2]; x1, rx1 = x1D[i % 2]; junk2, rjunk2 = jkD[i % 2]; s8, rs8 = s8D[i % 2]
        P.dma(ym, ymix[tsl, :], ["ymix"], [rym], q="sp")
        P.dma(xt, x_own[tsl, :], [], [rxt], q="pool")
        pT, rpT = bank(0, 2)
        for k in range(8):
            P.mm(pT[:, k // 4, (k % 4) * 128:(k % 4) * 128 + 128], ym[:, k * 128:(k + 1) * 128], identb, True, True, [rym, rCB], rpT)
        V("act", "copy", yT[:, 0:4, :], pT[:, 0, :].rearrange("p (k n) -> p k n", k=4), r=rpT, w=[ryT])
        V("act", "copy", yT[:, 4:8, :], pT[:, 1, :].rearrange("p (k n) -> p k n", k=4), r=rpT, w=[ryT])
        pX, rpX = bank(2, 2)
        for hb in range(2):
            for k in range(8):
                P.mm(pX[:, hb, :], yT[:, k, :], Wo[:, k, hb * 512:(hb + 1) * 512], k == 0, k == 7, [ryT, rWo], rpX)
        V("dve", "tensor_tensor", x1, pX.rearrange("p b n -> p (b n)"), xt, ALU.add, r=rpX + [rxt], w=[rx1])
        P.dma(x1s[tsl, :], x1, [rx1], ["x1s"], q="pool")
        P.act(junk2, x1, AF.Square, [rx1], [rjunk2, rs8], accum_out=s8[:, 0:1])
        P.act(s8[:, 1:2], s8[:, 0:1], AF.Sqrt, [rs8], [rs8], scale=1.0 / D, bias=EPS)
        V("dve", "reciprocal", s8[:, 2:3], s8[:, 1:2], r=[rs8], w=[rs8])
        V("dve", "scalar_tensor_tensor", junk2, x1, s8[:, 2:3], s2_bc, ALU.mult, ALU.mult, r=[rx1, rs8, rmodr], w=[rjunk2])
        V("dve", "tensor_tensor", tb16, junk2, sh2_bc, ALU.add, r=[rjunk2, rmodr], w=[rtb16])
        pT2, rpT2 = bank(4, 2)
        for k in range(8):
            P.mm(pT2[:, k // 4, (k % 4) * 128:(k % 4) * 128 + 128], tb16[:, k * 128:(k + 1) * 128], identb, True, True, [rtb16, rCB], rpT2)
        V("act", "copy", tT[:, 0:4, tsl], pT2[:, 0, :].rearrange("p (k n) -> p k n", k=4), r=rpT2, w=[rtT])
        V("act", "copy", tT[:, 4:8, tsl], pT2[:, 1, :].rearrange("p (k n) -> p k n", k=4), r=rpT2, w=[rtT])
        pL, rpL = bank(6)
        for k in range(8):
            P.mm(pL[:, 0:36], tT[:, k, tsl], Wr[:, k, :], k == 0, k == 7, [rtT, rWr], rpL)
        V("dve", "tensor_tensor", lg_all[:, i, :], pL[:, 0:36], rbb, ALU.add, r=rpL + [rrbb], w=[rlg_all])

    AXX = mybir.AxisListType.X
    NT = 16
    gl = lg_all[:, :, 0:4]
    el = lg_all[:, :, 4:36].rearrange("p t (g e) -> p t g e", g=4)

    def ra(shape, name):
        return A.alloc(shape, F32, name)

    gmax, rgmax = ra([128, NT], "gmax")
    ohg, rohg = ra([128, NT, 4], "ohg")
    gd, rgd = ra([128, NT, 4], "gd")
    pg, rpg_ = ra([128, NT], "pg")
    prod, rprod = ra([128, NT, 4, 8], "prod")
    esel, resel = ra([128, NT, 8], "esel")
    m1, rm1 = ra([128, NT], "m1")
    m2, rm2 = ra([128, NT], "m2")
    oh1, roh1 = ra([128, NT, 8], "oh1")
    msk, rmsk = ra([128, NT, 8], "msk")
    oh2, roh2 = ra([128, NT, 8], "oh2")
    w1, rw1 = ra([128, NT], "w1")
    w2, rw2 = ra([128, NT], "w2")
    wig, rwig = ra([128, NT, 8], "wig")
    V("dve", "reduce_max", gmax, gl, AXX, r=[rlg_all], w=[rgmax])
    V("dve", "tensor_tensor", ohg, gl, bc(gmax, 2, 4), ALU.is_equal, r=[rlg_all, rgmax], w=[rohg])
    V("dve", "tensor_tensor", gd, gl, bc(gmax, 2, 4), ALU.subtract, r=[rlg_all, rgmax], w=[rgd])
    P.act(gd, gd, AF.Exp, [rgd], [rgd])
    V("dve", "reduce_sum", pg, gd, AXX, r=[rgd], w=[rpg_])
    V("dve", "reciprocal", pg, pg, r=[rpg_], w=[rpg_])
    V("dve", "tensor_tensor", prod, el, bc(ohg, 3, 8), ALU.mult, r=[rlg_all, rohg], w=[rprod])
    V("dve", "reduce_sum", esel, prod.rearrange("p t g e -> p t e g"), AXX, r=[rprod], w=[resel])
    V("dve", "reduce_max", m1, esel, AXX, r=[resel], w=[rm1])
    V("dve", "tensor_tensor", oh1, esel, bc(m1, 2, 8), ALU.is_equal, r=[resel, rm1], w=[roh1])
    V("dve", "scalar_tensor_tensor", msk, oh1, -1e30, esel, ALU.mult, ALU.add, r=[roh1, resel], w=[rmsk])
    V("dve", "reduce_max", m2, msk, AXX, r=[rmsk], w=[rm2])
    V("dve", "tensor_tensor", oh2, msk, bc(m2, 2, 8), ALU.is_equal, r=[rmsk, rm2], w=[roh2])
    V("dve", "tensor_tensor", w1, m1, m2, ALU.subtract, r=[rm1, rm2], w=[rw1])
    P.act(w1, w1, AF.Sigmoid, [rw1], [rw1])
    V("dve", "tensor_tensor", w1, w1, pg, ALU.mult, r=[rw1, rpg_], w=[rw1])
    V("dve", "tensor_tensor", w2, pg, w1, ALU.subtract, r=[rpg_, rw1], w=[rw2])
    V("dve", "tensor_tensor", wig, oh1, bc(w1, 2, 8), ALU.mult, r=[roh1, rw1], w=[rwig])
    V("dve", "tensor_tensor", oh2, oh2, bc(w2, 2, 8), ALU.mult, r=[roh2, rw2], w=[roh2])
    V("dve", "tensor_tensor", wig, wig, oh2, ALU.add, r=[rwig, roh2], w=[rwig])
    V("dve", "tensor_tensor", comb.rearrange("p t (g e) -> p t g e", g=4), bc(ohg, 3, 8), bc(wig, 2, 4), ALU.mult, r=[rohg, rwig], w=[rcomb])

    xt, rxt, x1, rx1, junk2, rjunk2, s8, rs8 = xt_f, rxt_f, x1_f, rx1_f, junk2_f, rjunk2_f, s8_f, rs8_f
    P.fence()
    A.release(mD)
    acc, racc = A.alloc([128, 16, 1024], F32, "acc")
    for i in range(16):
        P.dma(acc[:, i, :], x1s[i * 128:(i + 1) * 128, :], ["x1s"], [racc + "_%d" % i], q=("sp" if i % 2 == 0 else "act"))
    mE = A.mark()
    wst32 = [A.alloc([128, 8, 256], F32, "wst32_%d" % i) for i in range(3)]
    Wg_ = [A.alloc([128, 8, 256], BF16, "Wg%d" % i) for i in range(2)]
    Wu_ = [A.alloc([128, 8, 256], BF16, "Wu%d" % i) for i in range(2)]
    Wd_ = [A.alloc([128, 2, 1024], BF16, "Wd%d" % i) for i in range(2)]
    sg = [[A.alloc([128, 512], BF16, "sg%d_%d" % (i, j)) for j in range(2)] for i in range(2)]
    hTt = [[A.alloc([128, 512], BF16, "hT%d_%d" % (i, j)) for j in range(2)] for i in range(2)]
    Wcur = {}

    def moe_gu(e, tb, par):
        if tb == 0:
            Wg, rWg = Wg_[e % 2]
            Wu, rWu = Wu_[e % 2]
            Wd, rWd = Wd_[e % 2]
            s0, rs0 = wst32[0]
            s1_, rs1_ = wst32[1]
            s2_, rs2_ = wst32[2]
            P.dma(s0, wg[e].rearrange("(k p) f -> p k f", p=128), [], [rs0], q="sp")
            V("pool", "tensor_copy", Wg, s0, r=[rs0], w=[rWg])
            P.dma(s1_, wu[e].rearrange("(k p) f -> p k f", p=128), [], [rs1_], q="sp")
            V("pool", "tensor_copy", Wu, s1_, r=[rs1_], w=[rWu])
            s2v = s2_.rearrange("p k f -> p (k f)").rearrange("p (c n) -> p c n", c=2)
            P.dma(s2v, wd[e].rearrange("(c p) n -> p c n", p=128), [], [rs2_], q="sp")
            V("pool", "tensor_tensor", Wd, s2v, bc(g2_bc, 1, 2), ALU.mult, r=[rs2_, rmodr], w=[rWd])
        Wg, rWg = Wg_[e % 2]
        Wu, rWu = Wu_[e % 2]
        tbs = slice(tb * 512, (tb + 1) * 512)
        for fh in range(2):
            pg, rpg = bank(fh)
            pu, rpu = bank(2 + fh)
            for k in range(8):
                P.mm(pg, Wg[:, k, fh * 128:(fh + 1) * 128], tT[:, k, tbs], k == 0, k == 7, [rWg, rtT], rpg)
            for k in range(8):
                P.mm(pu, Wu[:, k, fh * 128:(fh + 1) * 128], tT[:, k, tbs], k == 0, k == 7, [rWu, rtT], rpu)
            P.act(sg[fh][par][0], pg, AF.Silu, rpg, [sg[fh][par][1]])
            V("dve", "tensor_tensor", hTt[fh][par][0], sg[fh][par][0], pu, ALU.mult, r=[sg[fh][par][1]] + rpu, w=[hTt[fh][par][1]])

    def moe_down(e, tb, par):
        Wd, rWd = Wd_[e % 2]
        for st_ in range(4):
            ti = tb * 4 + st_
            po, rpo = bank(4 + 2 * (st_ % 2), 2)
            for dh in range(2):
                for fc in range(2):
                    P.mm(po[:, dh, :], hTt[fc][par][0][:, st_ * 128:(st_ + 1) * 128], Wd[:, fc, dh * 512:(dh + 1) * 512], fc == 0, fc == 1, [hTt[fc][par][1], rWd], rpo)
            ra_ = racc + "_%d" % ti
            V("dve", "scalar_tensor_tensor", acc[:, ti, :], po.rearrange("p b n -> p (b n)"), comb[:, ti, e:e + 1], acc[:, ti, :], ALU.mult, ALU.add, r=rpo + [rcomb, ra_], w=[ra_])

    blks = [(e, tb) for e in range(32) for tb in range(4)]
    moe_gu(blks[0][0], blks[0][1], 0)
    for i_, (e, tb) in enumerate(blks):
        if i_ + 1 < len(blks):
            moe_gu(blks[i_ + 1][0], blks[i_ + 1][1], (i_ + 1) % 2)
        moe_down(e, tb, i_ % 2)

    P.fence()
    A.release(mE)
    jkF = [A.alloc([128, 1024], F32, "jkF%d" % i) for i in range(2)]
    s8F = [A.alloc([128, 8], F32, "s8F%d" % i) for i in range(2)]
    for i in range(16):
        tsl = slice(i * 128, (i + 1) * 128)
        junk2, rjunk2 = jkF[i % 2]; s8, rs8 = s8F[i % 2]
        x2, rx2 = acc[:, i, :], racc + "_%d" % i
        P.act(junk2, x2, AF.Square, [rx2], [rjunk2, rs8], accum_out=s8[:, 0:1])
        P.act(s8[:, 1:2], s8[:, 0:1], AF.Sqrt, [rs8], [rs8], scale=1.0 / D, bias=EPS)
        V("dve", "reciprocal", s8[:, 2:3], s8[:, 1:2], r=[rs8], w=[rs8])
        V("dve", "scalar_tensor_tensor", junk2, x2, s8[:, 2:3], fnw, ALU.mult, ALU.mult, r=[rx2, rs8, rfnw], w=[rjunk2])
        P.dma(out[tsl, :], junk2, [rjunk2], ["out"], q=("sp" if i % 2 == 0 else "act"))

    P.finalize(st)
    st.close()
    print("sem counts", P.sem_counts, "nops", len(P.ops), "sim_us", P.sim_time)
    return nc


_NC_CACHE = {}


def _host_inputs(inp):
    f = lambda a: np.ascontiguousarray(np.asarray(a, dtype=np.float32))
    x = f(inp["x"]); c = f(inp["c"]); ctx = f(inp["ctx"]); c_ctx = f(inp["c_ctx"])
    L = 0
    w_mod = f(inp["w_mod"][L]); b_mod = f(inp["b_mod"][L]); n1w = f(inp["norm1_w"][L]); w_in = f(inp["w_in"][L])
    eye = np.eye(128, dtype=np.float32)
    k = np.arange(128)
    Uf = (k[:, None] <= k[None, :]).astype(np.float32)
    Ub = (k[:, None] >= k[None, :]).astype(np.float32)
    MNf = np.where(k[None, :] >= k[:, None], 0.0, -30000.0).astype(np.float32)
    MNb = np.where(k[None, :] <= k[:, None], 0.0, -30000.0).astype(np.float32)
    cst = f(np.stack([eye, Uf, Ub, np.ones((128, 128), np.float32), MNf, MNb], axis=1))
    col8 = lambda v: f(v.reshape(8, 128).T)
    bmod_col = f(np.concatenate([col8(b_mod[0:1024]), col8(b_mod[1024:2048])], axis=1))
    w_r = f(np.concatenate([inp["moe_rg_w"][L], inp["moe_re_w"][L]], axis=1))
    rb_row = f(np.concatenate([inp["moe_rg_b"][L], inp["moe_re_b"][L]])[None, :])
    wqk_in = f(inp["ml_w_qk"][L])
    wqk = np.zeros((2, 4, 128, 128), np.float32)
    for s in range(2):
        for n in range(128):
            ch, o = divmod(n * 4, 128)
            wqk[s, ch, o:o + 4, o:o + 4] = wqk_in[s, n]
    shared = dict(w_mod=w_mod, bmod_col=bmod_col, bmod_row=f(b_mod[None, :]), n1w_col=col8(n1w),
                  ssd_cb=f(inp["ssd_conv_b"][L].reshape(8, 128).T), ml_cb=f(inp["ml_conv_b"][L].reshape(4, 128).T),
                  wqk=wqk, w_out=f(inp["w_out"][L]), n2w_row=f(inp["norm2_w"][L][None, :]), w_r=w_r, rb_row=rb_row,
                  wg=f(inp["moe_w_gate"][L]), wu=f(inp["moe_w_up"][L]), wd=f(inp["moe_w_down"][L]),
                  fnw_row=f(inp["final_norm_w"][None, :]), cst=cst)
    maps = []
    for core in range(8):
        b, h = divmod(core, 2)
        rev = h == 1
        xb = x[b]
        cb = ctx[b]
        if rev:
            xb = xb[::-1]
            cb = cb[::-1]
        def blk(a_t):
            n_ = a_t.shape[1]
            return f(a_t.reshape(8, 128, n_ // 256, 256).transpose(2, 1, 0, 3).reshape(n_ // 256, 128, 8 * 256))
        xT_r = blk(xb.T)
        xT_c = blk(xb.reshape(64, 64, D).transpose(1, 0, 2).reshape(T, D).T)
        ctxT = blk(cb.T)
        x_own = f(xb[:TOWN])
        ccol = f(np.stack([c[b].reshape(8, 128).T, c_ctx.reshape(8, 128).T], axis=2))
        wi = w_in.copy()
        scw = f(inp["ssd_conv_w"][L]); mcw = f(inp["ml_conv_w"][L])
        dtb = f(inp["ssd_dt_bias"][L]); alog = f(inp["ssd_a_log"][L]); gb = f(inp["ml_gate_b"][L])
        if rev:
            wi[:, 1536:1552] = np.concatenate([w_in[:, 1544:1552], w_in[:, 1536:1544]], axis=1)
            g0 = 1552 + 1536
            gcols = w_in[:, g0:g0 + 16].reshape(D, 2, 2, 4)[:, :, ::-1, :].reshape(D, 16)
            wi[:, g0:g0 + 16] = gcols
            scw = scw[::-1]; mcw = mcw[::-1]
            dtb = dtb[::-1]; alog = alog[::-1]; gb = gb[:, ::-1, :]
        ssd_cw = f(scw.T.reshape(8, 128, 5).transpose(1, 0, 2))
        ml_cw = f(mcw.T.reshape(4, 128, 5).transpose(1, 0, 2))
        ssd_row = f(np.concatenate([dtb.reshape(-1), alog.reshape(-1), inp["ssd_d"][L], inp["ssd_norm_w"][L]])[None, :])
        ml_row = f(np.concatenate([gb.reshape(-1), inp["ml_norm_w"][L], inp["ml_skip"][L]])[None, :])
        m = dict(shared)
        m.update(xT_r=xT_r, xT_c=xT_c, ctxT=ctxT, x_own=x_own, ccol=ccol, w_in=f(wi), ssd_cw=ssd_cw, ml_cw=ml_cw,
                 ssd_row=ssd_row, ml_row=ml_row)
        maps.append(m)
    return maps


def kernel(**inputs):
    maps = _host_inputs(inputs)
    if "nc" not in _NC_CACHE:
        _NC_CACHE["nc"] = build_nc()
    nc = _NC_CACHE["nc"]
    res = run_bass_kernel_spmd(nc, maps, core_ids=list(range(8)))
    outp = np.zeros((4, T, D), np.float32)
    for core in range(8):
        b, h = divmod(core, 2)
        o = np.asarray(res.results[core]["out"], dtype=np.float32)
        if h == 0:
            outp[b, :TOWN] = o
        else:
            outp[b, TOWN:] = o[::-1]
    kernel.last_results = res
    return outp
```

```python
import numpy as np
from contextlib import ExitStack
import concourse.bass as bass
import concourse.mybir as mybir
from concourse.bass_utils import run_bass_kernel_spmd

F32 = mybir.dt.float32
BF16 = mybir.dt.bfloat16
AF = mybir.ActivationFunctionType
ALU = mybir.AluOpType

D = 1024
T = 4096
TC = 256
TOWN = 2048
EPS = 1e-6
DEBUG = set()


class _Op:
    __slots__ = ("eng", "meth", "args", "kw", "reads", "writes", "dma", "deps", "odeps", "signal", "sem", "val", "prewait", "idx")


class Prog:
    ENGS = ("pe", "act", "dve", "pool", "sp")

    def __init__(self, nc, n_dma_sems=12):
        self.nc = nc
        self.ops = []
        self.n_dma_sems = n_dma_sems
        self.fence_res = None
        self.all_res = set()

    def I(self, eng, meth, args, kw=None, r=(), w=(), dma=False):
        o = _Op()
        o.eng = eng; o.meth = meth; o.args = args; o.kw = kw or {}
        r = tuple(r); w = tuple(w)
        pb_ = tuple(x for x in r if len(x) == 2 and x[0] == "b" and x[1].isdigit())
        if pb_:
            r = tuple(x for x in r if x not in pb_)
            w = w + tuple(x for x in pb_ if x not in w)
        o.reads = r + ((self.fence_res,) if self.fence_res else ())
        o.writes = w
        o.dma = dma; o.deps = []; o.signal = dma; o.sem = None; o.val = 0; o.prewait = None
        o.idx = len(self.ops)
        self.ops.append(o)
        self.all_res.update(o.reads); self.all_res.update(o.writes)
        return o

    def fence(self):
        name = "__fence%d" % len(self.ops)
        res = [x for x in self.all_res if not x.startswith("__fence")]
        self.I("pool", "nop", (), {}, r=(), w=res + [name])
        self.fence_res = name

    def mm(self, out, lhsT, rhs, start, stop, r, w):
        return self.I("pe", "matmul", (out, lhsT, rhs), dict(start=start, stop=stop), r, w)

    def dma(self, out, in_, r, w, q="sp"):
        return self.I(q, "dma_start", (), dict(out=out, in_=in_), r, w, dma=True)

    def act(self, out, in_, func, r, w, **kw):
        return self.I("act", "activation", (out, in_, func), kw, r, w)

    def _dur(self, o):
        def free(ap):
            n = 1
            for x in ap.shape[1:]:
                n *= x
            return n
        try:
            if o.dma:
                ap = o.kw["out"]
                return 2.0 + free(ap) * ap.shape[0] * (4 if ap.dtype == F32 else 2) / 150e3
            if o.meth == "matmul":
                out, lhsT = o.args[0], o.args[1]
                return 0.03 + max(free(out), 32) / 2000.0 * (4 if lhsT.dtype == F32 else 1)
            if o.meth == "nop":
                return 0.05
            ap = o.args[0]
            n = free(ap)
            if o.meth == "reciprocal":
                return 0.15 + n / 160.0
            if o.eng == "act":
                return 0.25 + n / 1200.0
            if o.eng == "pool":
                return 0.3 + n / 500.0
            return 0.15 + n / 960.0
        except Exception:
            return 0.5

    def finalize(self, stack):
        nc = self.nc
        ops = self.ops
        last_w = {}
        readers = {}
        for o in ops:
            deps = {}
            odeps = {}

            def add(d, raw):
                if d is None or d is o:
                    return
                odeps[d.idx] = d
                same = (d.eng == o.eng) and not d.dma and not o.dma
                if same and not raw and o.eng == "pe":
                    return
                deps[d.idx] = d

            for x in o.reads:
                add(last_w.get(x), True)
            for x in o.writes:
                add(last_w.get(x), False)
                for rd in readers.get(x, ()):
                    add(rd, False)
            o.deps = list(deps.values())
            o.odeps = list(odeps.values())
            for d in o.deps:
                d.signal = True
            for x in o.reads:
                readers.setdefault(x, []).append(o)
            for x in o.writes:
                last_w[x] = o
                readers[x] = []
        per0 = {e: [o for o in ops if o.eng == e] for e in self.ENGS}
        W = 400
        fin_t = {}
        eng_free = {e: 0.0 for e in self.ENGS}
        pending = {e: list(per0[e]) for e in self.ENGS}
        order = {e: [] for e in self.ENGS}
        durs = {o.idx: self._dur(o) for o in ops}
        remaining = len(ops)
        while remaining:
            best = None
            for e in self.ENGS:
                lst = pending[e]
                lim = min(W, len(lst))
                for j in range(lim):
                    o = lst[j]
                    t = eng_free[e]
                    ok = True
                    for d in o.odeps:
                        ft = fin_t.get(d.idx)
                        if ft is None:
                            ok = False
                            break
                        if ft > t:
                            t = ft
                    if not ok:
                        continue
                    key = (t, o.idx)
                    if best is None or key < best[0]:
                        best = (key, e, j, o, t)
                    if t <= eng_free[e]:
                        break
            assert best is not None, "scheduler stuck"
            _, e, j, o, t = best
            pending[e].pop(j)
            order[e].append(o)
            if o.dma:
                eng_free[e] = t + 0.15
                fin_t[o.idx] = t + durs[o.idx]
            else:
                eng_free[e] = t + durs[o.idx]
                fin_t[o.idx] = t + durs[o.idx] + 0.1
            remaining -= 1
        self.sim_time = max(fin_t.values()) if fin_t else 0.0
        pos = {}
        for e in self.ENGS:
            for i_, o in enumerate(order[e]):
                pos[o.idx] = i_
        for o in ops:
            if not o.dma:
                o.signal = False
        for o in ops:
            bye = {}
            keep = []
            for d in o.deps:
                if d.dma:
                    keep.append(d)
                else:
                    c_ = bye.get(d.eng)
                    if c_ is None or pos[d.idx] > pos[c_.idx]:
                        bye[d.eng] = d
            keep.extend(bye.values())
            o.deps = keep
            for d in keep:
                d.signal = True
        esem = {}
        for e in ("pe", "act", "dve", "pool"):
            esem[e] = stack.enter_context(nc.semaphore("s_" + e))
        dsems = {}
        for q in ("sp", "pool", "act"):
            if any(o.dma and o.eng == q for o in ops):
                dsems[q] = [stack.enter_context(nc.semaphore("d_%s_%d" % (q, i))) for i in range(self.n_dma_sems)]
        cnt = {e: 0 for e in esem}
        dcnt = {q: [0] * self.n_dma_sems for q in dsems}
        drr = {q: 0 for q in dsems}
        for e in self.ENGS:
            for o in order[e]:
                if o.dma:
                    i = drr[o.eng]
                    drr[o.eng] = (i + 1) % self.n_dma_sems
                    o.sem = dsems[o.eng][i]
                    if dcnt[o.eng][i] > 0:
                        o.prewait = (o.sem, dcnt[o.eng][i])
                    dcnt[o.eng][i] += 16
                    o.val = dcnt[o.eng][i]
                elif o.signal:
                    cnt[o.eng] += 1
                    o.sem = esem[o.eng]
                    o.val = cnt[o.eng]
        self.sem_counts = dict(cnt)
        per = order
        block = stack.enter_context(nc.Block())

        def emit(engname):
            lst = per[engname]

            def body(e):
                known = {}
                for o in lst:
                    waits = [(d.sem, d.val) for d in o.deps]
                    if o.prewait is not None:
                        waits.append(o.prewait)
                    for (s, v) in waits:
                        k = id(s)
                        if known.get(k, 0) >= v:
                            continue
                        e.wait_ge(s, v)
                        known[k] = v
                    ins = getattr(e, o.meth)(*o.args, **o.kw)
                    if o.dma:
                        ins.then_inc(o.sem, 16)
                    elif o.signal:
                        ins.then_inc(o.sem, 1)
                if engname in dsems:
                    for i, s in enumerate(dsems[engname]):
                        if dcnt[engname][i] > 0:
                            e.wait_ge(s, dcnt[engname][i])

            return body

        if per["pe"]:
            block.tensor(emit("pe"))
        if per["act"]:
            block.scalar(emit("act"))
        if per["dve"]:
            block.vector(emit("dve"))
        if per["pool"]:
            block.gpsimd(emit("pool"))
        if per["sp"]:
            block.sync(emit("sp"))


class Arena:
    def __init__(self, ap, nwords):
        self.ap = ap
        self.n = nwords
        self.off = 0
        self.cnt = 0

    def alloc(self, shape, dt=F32, name=None):
        free = 1
        for s in shape[1:]:
            free *= s
        words = free if dt == F32 else (free + 1) // 2
        words += words & 1
        assert self.off + words <= self.n, "arena overflow %d + %d > %d" % (self.off, words, self.n)
        v = self.ap[0:shape[0], self.off:self.off + words]
        self.off += words
        if dt != F32:
            v = v.bitcast(dt)[:, 0:free]
        if len(shape) > 2:
            names = ["a%d" % i for i in range(len(shape) - 1)]
            pat = "p (%s) -> p %s" % (" ".join(names), " ".join(names))
            v = v.rearrange(pat, **{n: s for n, s in zip(names[:-1], shape[1:-1])})
        self.cnt += 1
        return v, (name or "t") + "#%d" % self.cnt

    def mark(self):
        return self.off

    def release(self, m):
        self.off = m


def bc(ap, axis, n):
    v = ap.unsqueeze(axis)
    shp = list(v.shape)
    shp[axis] = n
    return v.to_broadcast(shp)


def build_nc():
    nc = bass.Bass("TRN2", target_bir_lowering=False)

    def din(name, shape, dt=F32):
        return nc.dram_tensor(name, list(shape), dt, kind="ExternalInput").ap()

    def scr(name, shape, dt=F32):
        kind = "ExternalOutput" if name in DEBUG else "Internal"
        return nc.dram_tensor(name, list(shape), dt, kind=kind).ap()

    xT = {"ssd": din("xT_r", [T // 256, 128, 8 * 256]), "ml": din("xT_c", [T // 256, 128, 8 * 256])}
    ctxT = din("ctxT", [1, 128, 8 * 256])
    x_own = din("x_own", [TOWN, D])
    ccol = din("ccol", [128, 8, 2])
    w_mod = din("w_mod", [D, 6 * D])
    bmod_col = din("bmod_col", [128, 16])
    bmod_row = din("bmod_row", [1, 6 * D])
    n1w_col = din("n1w_col", [128, 8])
    w_in = din("w_in", [D, 3104])
    ssd_cw = din("ssd_cw", [128, 8, 5])
    ssd_cb = din("ssd_cb", [128, 8])
    ssd_row = din("ssd_row", [1, 16 + 16 + 8 + 512])
    ml_cw = din("ml_cw", [128, 4, 5])
    ml_cb = din("ml_cb", [128, 4])
    wqk = din("wqk", [2, 4, 128, 128])
    ml_row = din("ml_row", [1, 16 + 512 + 512])
    w_out = din("w_out", [D, D])
    n2w_row = din("n2w_row", [1, D])
    w_r = din("w_r", [D, 36])
    rb_row = din("rb_row", [1, 36])
    wg = din("wg", [32, D, 256])
    wu = din("wu", [32, D, 256])
    wd = din("wd", [32, 256, D])
    fnw_row = din("fnw_row", [1, D])
    cst = din("cst", [128, 6, 128])
    out = nc.dram_tensor("out", [TOWN, D], F32, kind="ExternalOutput").ap()

    convT = {("ssd", "lat"): scr("cv_ssd_lat", [8, 128, T], BF16), ("ssd", "ctx"): scr("cv_ssd_ctx", [8, 128, TC], BF16),
             ("ml", "lat"): scr("cv_ml_lat", [4, 128, T], BF16), ("ml", "ctx"): scr("cv_ml_ctx", [4, 128, TC], BF16)}
    z_tm = scr("z_tm", [TOWN, 512], BF16)
    v_tm = {"lat": scr("v_lat", [T, 512], BF16), "ctx": scr("v_ctx", [TC, 512], BF16)}
    o_tm = scr("o_tm", [T, 512], BF16)
    yf_ssd = scr("yf_ssd", [TOWN, 512], F32)
    hf_ml = scr("hf_ml", [T, 512], F32)
    ymix = scr("ymix", [TOWN, D], BF16)
    x1s = scr("x1s", [TOWN, D], F32)
    modrow = scr("modrow", [1, 4 * D], F32)

    st = ExitStack()
    NW = 53000
    arena_t = st.enter_context(nc.sbuf_tensor("arena", [128, NW], F32))
    A = Arena(arena_t, NW)
    PS = st.enter_context(nc.psum_tensor("ps", [128, 8, 512], F32))
    P = Prog(nc)

    def bank(i, n=1):
        if n == 1:
            return PS[:, i, :], ["b%d" % i]
        return PS[:, i:i + n, :], ["b%d" % j for j in range(i, i + n)]

    def V(eng, meth, *args, r=(), w=(), **kw):
        return P.I(eng, meth, args, kw, r, w)

    C32, rC32 = A.alloc([128, 6, 128], F32, "C32")
    CB, rCB = A.alloc([128, 6, 128], BF16, "CB")
    P.dma(C32, cst, [], [rC32])
    V("dve", "tensor_copy", CB, C32, r=[rC32], w=[rCB])
    identb, Ub, onesb, MNb_ = CB[:, 0, :], (CB[:, 1, :], CB[:, 2, :]), CB[:, 3, :], (CB[:, 4, :], CB[:, 5, :])
    U32 = (C32[:, 1, :], C32[:, 2, :])
    ones32 = C32[:, 3, :]
    negones, rnegones = A.alloc([128, 128], BF16, "negones")
    V("dve", "tensor_scalar", negones, onesb, -1.0, None, ALU.mult, r=[rCB], w=[rnegones])
    MNrep = []
    for d_ in range(2):
        t_, r_ = A.alloc([128, 4, 128], BF16, "MNrep")
        V("dve", "tensor_copy", t_, bc(MNb_[d_], 1, 4), r=[rCB], w=[r_])
        MNrep.append((t_, r_))

    def bcrow(src_ap, n, name, q="sp"):
        t_, r_ = A.alloc([128, n], F32, name)
        P.dma(t_, src_ap.partition_broadcast(128), [], [r_], q=q)
        return t_, r_

    ssdrow, rssdrow = bcrow(ssd_row, 552, "ssdrow")
    mlrow, rmlrow = bcrow(ml_row, 1040, "mlrow")
    dtb_bc = ssdrow[:, 0:16]
    aneg_bc, raneg = A.alloc([128, 16], F32, "aneg")
    P.act(aneg_bc, ssdrow[:, 16:32], AF.Exp, [rssdrow], [raneg])
    V("dve", "tensor_scalar", aneg_bc, aneg_bc, -1.0, None, ALU.mult, r=[raneg], w=[raneg])
    Dh_bc = ssdrow[:, 32:40]
    ssdnw_bc = ssdrow[:, 40:552]
    gb_bc = mlrow[:, 0:16]
    mlnw_bc = mlrow[:, 16:528]
    mlskip_bc = mlrow[:, 528:1040]

    dt_all = {}; a_all = {}; gi_all = {}; af_all = {}
    for tag, nt in (("lat", 32), ("ctx", 2)):
        dt_all[tag] = A.alloc([128, nt, 16], F32, "dt_" + tag)
        a_all[tag] = A.alloc([128, nt, 16], F32, "a_" + tag)
        gi_all[tag] = A.alloc([128, nt, 8], F32, "gi_" + tag)
        af_all[tag] = A.alloc([128, nt, 8], F32, "af_" + tag)

    cols, rcols = A.alloc([128, 4, 8], F32, "cols")
    m0 = A.mark()
    cc, rcc = A.alloc([128, 8, 2], F32, "cc")
    sil, rsil = A.alloc([128, 8, 2], F32, "sil")
    P.dma(cc, ccol, [], [rcc])
    P.act(sil, cc, AF.Silu, [rcc], [rsil])
    silT, rsilT = A.alloc([128, 8, 64], F32, "silT")
    V("dve", "memset", silT, 0.0, r=[], w=[rsilT])
    V("dve", "tensor_copy", silT[:, :, 0:1], sil[:, :, 0:1], r=[rsil, rsilT], w=[rsilT])
    V("dve", "tensor_copy", silT[:, :, 32:33], sil[:, :, 1:2], r=[rsil, rsilT], w=[rsilT])
    bmc, rbmc = A.alloc([128, 16], F32, "bmc")
    n1c, rn1c = A.alloc([128, 8], F32, "n1c")
    P.dma(bmc, bmod_col, [], [rbmc])
    P.dma(n1c, n1w_col, [], [rn1c])
    brow, rbrow = A.alloc([1, 4 * D], F32, "brow")
    P.dma(brow, bmod_row[:, 2 * D:6 * D], [], [rbrow])
    rowm, rrowm = A.alloc([64, 6 * D], F32, "rowm")
    wst = [A.alloc([128, 8, 512], F32, "wst%d" % i) for i in range(4)]
    wmod_v = w_mod.rearrange("(k p) n -> p k n", p=128)
    for nb_ in range(12):
        ws, rws = wst[nb_ % 4]
        P.dma(ws, wmod_v[:, :, nb_ * 512:(nb_ + 1) * 512], [], [rws], q=("sp" if nb_ % 2 == 0 else "act"))
        pr, rpr = bank(nb_ % 2)
        for k in range(8):
            P.mm(pr[0:64, :], silT[:, k, :], ws[:, k, :], k == 0, k == 7, [rws, rsilT], rpr)
        rr_ = rrowm + "_%d" % nb_
        if nb_ % 2 == 0:
            V("act", "copy", rowm[:, nb_ * 512:(nb_ + 1) * 512], pr[0:64, :], r=rpr, w=[rr_])
        else:
            V("dve", "tensor_copy", rowm[:, nb_ * 512:(nb_ + 1) * 512], pr[0:64, :], r=rpr, w=[rr_])
    pc, rpc = bank(2)
    for src_ in range(2):
        pp_ = 32 * src_
        for vec in range(2):
            for j in range(8):
                idx = (src_ * 2 + vec) * 8 + j
                c0_ = vec * D + j * 128
                P.mm(pc[:, idx:idx + 1], rowm[pp_:pp_ + 1, c0_:c0_ + 128], C32[pp_:pp_ + 1, 3, 0:1], True, True,
                     [rrowm + "_%d" % (c0_ // 512), rC32], rpc)
    modc, rmodc = A.alloc([128, 2, 2, 8], F32, "modc")
    V("dve", "tensor_copy", modc, pc[:, 0:32].rearrange("p (s v j) -> p s v j", s=2, v=2), r=rpc, w=[rmodc])
    for j in range(2):
        V("dve", "tensor_tensor", cols[:, 2 * j + 1, :], modc[:, j, 0, :], bmc[:, 0:8], ALU.add, r=[rmodc, rbmc], w=[rcols])
        V("dve", "tensor_tensor", cols[:, 2 * j, :], modc[:, j, 1, :], bmc[:, 8:16], ALU.add, r=[rmodc, rbmc], w=[rcols])
        V("dve", "scalar_tensor_tensor", cols[:, 2 * j, :], cols[:, 2 * j, :], 1.0, n1c, ALU.add, ALU.mult, r=[rcols, rn1c], w=[rcols])
    V("dve", "tensor_tensor", rowm[0:1, 2 * D:6 * D], rowm[0:1, 2 * D:6 * D], brow, ALU.add,
      r=[rrowm + "_%d" % i for i in range(4, 12)] + [rbrow], w=[rrowm + "_g"])
    P.dma(modrow, rowm[0:1, 2 * D:6 * D], [rrowm + "_g"], ["modrow"], q="sp")
    P.fence()
    A.release(m0)

    win_v = w_in.rearrange("(k p) n -> p k n", p=128)

    def inproj_pass(mx):
        m1 = A.mark()
        col0 = 0 if mx == "ssd" else 1552
        nch = 8 if mx == "ssd" else 4
        cvc0 = 512 if mx == "ssd" else 0
        Wb, rWb = A.alloc([128, 8, 1552], BF16, "Wb")
        stgW = [A.alloc([128, 8, 512], F32, "stg%d" % i) for i in range(2)]
        stg = []
        for (t_, r_) in stgW:
            stg.append((t_[:, :, 0:256], r_ + "a"))
            stg.append((t_[:, :, 256:512], r_ + "b"))
        for blk in range(4):
            s_, rs_ = stgW[blk % 2]
            rsW = [rs_ + "a", rs_ + "b"]
            sv = s_[:, :, 0:388]
            P.dma(sv, win_v[:, :, col0 + blk * 388: col0 + (blk + 1) * 388], [], rsW, q=("sp" if blk % 2 == 0 else "pool"))
            if blk % 2 == 0:
                V("dve", "tensor_copy", Wb[:, :, blk * 388:(blk + 1) * 388], sv, r=rsW, w=[rWb])
            else:
                V("act", "copy", Wb[:, :, blk * 388:(blk + 1) * 388], sv, r=rsW, w=[rWb])
        cw_t, rcw = A.alloc([128, nch, 5], F32, "cw")
        cb_t, rcb = A.alloc([128, nch], F32, "cb")
        P.dma(cw_t, ssd_cw if mx == "ssd" else ml_cw, [], [rcw])
        P.dma(cb_t, ssd_cb if mx == "ssd" else ml_cb, [], [rcb])
        DG, rDG = A.alloc([128, nch, 5, 128], BF16, "DG")
        for j in range(nch):
            for tap in range(5):
                V("dve", "tensor_scalar", DG[:, j, tap, :], identb, cw_t[:, j, tap:tap + 1], None, ALU.mult, r=[rCB, rcw], w=[rDG + "_%d_%d" % (j, tap)])
        hT_l, rhT_l = A.alloc([128, 8, T], BF16, "hT")
        hT_c, rhT_c = A.alloc([128, 8, TC], BF16, "hTc")
        sq2 = [A.alloc([128, 8, 256], BF16, "sq%d" % i) for i in range(2)]
        rsb2 = [A.alloc([128, 256], F32, "rsb%d" % i) for i in range(2)]
        pre_l = [A.alloc([128, T + 4], BF16, "pre%d" % i) for i in range(2)]
        pre_c = [A.alloc([128, TC + 4], BF16, "prec%d" % i) for i in range(2)]
        cv_l = [A.alloc([128, T], BF16, "cv%d" % i) for i in range(2)]
        cv_c = [A.alloc([128, TC], BF16, "cvc%d" % i) for i in range(2)]
        ev = [A.alloc([128, 512], BF16, "ev%d" % i) for i in range(4)]
        sm4 = [A.alloc([128, 16], F32, "sm%d" % i) for i in range(4)]
        for (p_, rp_) in pre_l + pre_c:
            V("pool", "memset", p_, 0.0, r=[], w=[rp_])
        evi = [0]
        smi = [0]
        bbi = [0]
        sbi = [0]

        def big_bank():
            bbi[0] += 1
            return 6 + bbi[0] % 2

        def small_bank():
            sbi[0] += 1
            return sbi[0] % 2
        for tag in ("ctx", "lat"):
            hT, rhT = (hT_c, rhT_c) if tag == "ctx" else (hT_l, rhT_l)
            pre = pre_c if tag == "ctx" else pre_l
            cvs = cv_c if tag == "ctx" else cv_l
            Tn = TC if tag == "ctx" else T
            TB = 256
            src = ctxT if tag == "ctx" else xT[mx]
            scol = cols[:, 2, :] if tag == "ctx" else cols[:, 0, :]
            shcol = cols[:, 3, :] if tag == "ctx" else cols[:, 1, :]
            for tb in range(Tn // TB):
                s_, rs_ = stg[tb % 4]
                sq, rsq = sq2[tb % 2]
                rsb, rrsb = rsb2[tb % 2]
                xs = s_[:, :, 0:TB]
                P.dma(xs, src[tb].rearrange("p (k t) -> p k t", k=8), [], [rs_], q=("sp" if tb % 2 == 0 else "pool"))
                P.act(sq[:, :, 0:TB], xs, AF.Square, [rs_], [rsq])
                pb, rpb = bank(tb % 2)
                for k in range(8):
                    P.mm(pb[:, 0:TB], onesb, sq[:, k, 0:TB], k == 0, k == 7, [rCB, rsq], rpb)
                P.act(rsb[:, 0:TB], pb[:, 0:TB], AF.Ln, rpb, [rrsb], scale=1.0 / D, bias=EPS)
                P.act(rsb[:, 0:TB], rsb[:, 0:TB], AF.Exp, [rrsb], [rrsb], scale=-0.5)
                V("dve", "tensor_tensor", xs, xs, bc(rsb[:, 0:TB], 1, 8), ALU.mult, r=[rs_, rrsb], w=[rs_])
                for k in range(8):
                    hdst = hT[:, k, tb * TB:(tb + 1) * TB]
                    if k in (0, 1, 2):
                        P.act(hdst, xs[:, k, :], AF.Identity, [rs_, rcols], [rhT], scale=scol[:, k:k + 1], bias=shcol[:, k:k + 1])
                    else:
                        V("dve", "tensor_scalar", hdst, xs[:, k, :], scol[:, k:k + 1], shcol[:, k:k + 1], ALU.mult, ALU.add, r=[rs_, rcols], w=[rhT])
        for tag in ("ctx", "lat"):
            hT, rhT = (hT_c, rhT_c) if tag == "ctx" else (hT_l, rhT_l)
            pre = pre_c if tag == "ctx" else pre_l
            cvs = cv_c if tag == "ctx" else cv_l
            Tn = TC if tag == "ctx" else T
            TB = 256 if tag == "ctx" else 512
            xdep = [rhT_l] if tag == "ctx" else []
            cvd = convT[(mx, tag)]
            for j in range(nch):
                p_, rp_ = pre[j % 2]
                cv, rcv = cvs[j % 2]
                wc = cvc0 + j * 128
                for tb in range(Tn // TB):
                    pb, rpb = bank(2 + tb % 2)
                    for k in range(8):
                        P.mm(pb[:, 0:TB], Wb[:, k, wc:wc + 128], hT[:, k, tb * TB:(tb + 1) * TB], k == 0, k == 7, [rWb, rhT] + xdep, rpb)
                    if tb % 2 == 0:
                        V("act", "copy", p_[:, 2 + tb * TB:2 + (tb + 1) * TB], pb[:, 0:TB], r=rpb, w=[rp_])
                    else:
                        V("dve", "tensor_copy", p_[:, 2 + tb * TB:2 + (tb + 1) * TB], pb[:, 0:TB], r=rpb, w=[rp_])
                for tb in range(Tn // TB):
                    pb, rpb = bank(4 + tb % 2)
                    for tap in range(5):
                        P.mm(pb[:, 0:TB], DG[:, j, tap, :], p_[:, tb * TB + tap: tb * TB + tap + TB], tap == 0, tap == 4, [rDG + "_%d_%d" % (j, tap), rp_], rpb)
                    P.act(cv[:, tb * TB:(tb + 1) * TB], pb[:, 0:TB], AF.Silu, rpb + [rcb], [rcv], bias=cb_t[:, j:j + 1])
                P.dma(cvd[j], cv[:, 0:Tn], [rcv], ["cvd_%s_%s" % (mx, tag)], q=("sp" if j % 2 == 0 else "pool"))
            for tt in range(Tn // 128):
                tsl = slice(tt * 128, (tt + 1) * 128)

                def tm(colA, ncols, bk):
                    pb, rpb = bank(bk)
                    for k in range(8):
                        P.mm(pb[:, 0:ncols], hT[:, k, tsl], Wb[:, k, colA:colA + ncols], k == 0, k == 7, [rhT, rWb] + xdep, rpb)
                    return pb[:, 0:ncols], rpb

                def evac_store(pv, rpv, dst):
                    e_, re_ = ev[evi[0] % 4]
                    if evi[0] % 2 == 0:
                        V("act", "copy", e_, pv, r=rpv, w=[re_])
                    else:
                        V("dve", "tensor_copy", e_, pv, r=rpv, w=[re_])
                    P.dma(dst, e_, [re_], ["dst_" + dst.name if hasattr(dst, "name") else "dst"], q=("sp" if evi[0] % 2 == 0 else "pool"))
                    evi[0] += 1

                sm, rsm = sm4[smi[0] % 4]
                smi[0] += 1
                if mx == "ssd":
                    if tag == "lat" and tt < 16:
                        pv, rpv = tm(0, 512, big_bank())
                        evac_store(pv, rpv, z_tm[tsl, :])
                    pv, rpv = tm(1536, 16, small_bank())
                    dt_, rdt = dt_all[tag]
                    a_, ra = a_all[tag]
                    V("dve", "tensor_tensor", sm, pv, dtb_bc, ALU.add, r=rpv + [rssdrow], w=[rsm])
                    P.act(sm, sm, AF.Exp, [rsm], [rsm])
                    P.act(dt_[:, tt, :], sm, AF.Ln, [rsm], [rdt], bias=1.0)
                    V("dve", "tensor_tensor", a_[:, tt, :], dt_[:, tt, :], aneg_bc, ALU.mult, r=[rdt, raneg], w=[ra])
                else:
                    pv, rpv = tm(512, 512, big_bank())
                    evac_store(pv, rpv, v_tm[tag][tsl, :])
                    if tag == "lat":
                        pv, rpv = tm(1024, 512, big_bank())
                        evac_store(pv, rpv, o_tm[tsl, :])
                    pv, rpv = tm(1536, 16, small_bank())
                    gi_, rgi = gi_all[tag]
                    af_, raf = af_all[tag]
                    V("dve", "tensor_tensor", sm, pv, gb_bc, ALU.add, r=rpv + [rmlrow], w=[rsm])
                    V("dve", "tensor_copy", gi_[:, tt, :], sm[:, 0:8], r=[rsm], w=[rgi])
                    P.act(sm[:, 8:16], sm[:, 8:16], AF.Exp, [rsm], [rsm], scale=-1.0)
                    P.act(sm[:, 8:16], sm[:, 8:16], AF.Ln, [rsm], [rsm], bias=1.0)
                    V("dve", "tensor_scalar", af_[:, tt, :], sm[:, 8:16], -1.0, None, ALU.mult, r=[rsm], w=[raf])
        P.fence()
        A.release(m1)

    def scan_mixer(mx):
        m1 = A.mark()
        ssd = mx == "ssd"
        H = 8 if ssd else 4
        G = 2 if ssd else 4
        hg = H // G
        dv = 64 if ssd else 130
        slot = 64 if ssd else 256
        nb = 1 if ssd else 2
        nR = 2 if ssd else 1
        dva = dv + (dv & 1)
        def al3(name, dt_):
            t_, r_ = A.alloc([128, H, dva], dt_, name)
            return t_[:, :, 0:dv], r_
        S32 = [al3("S32_%d" % d_, F32) for d_ in range(2)]
        Sbf = [al3("Sbf_%d" % d_, BF16) for d_ in range(2)]
        XB = [A.alloc([128, 8 if ssd else 4, 128], BF16, "XB%d" % i) for i in range(3)]
        if ssd:
            xs_tm2 = [A.alloc([128, 8, 64], BF16, "xs_tm%d" % i) for i in range(3)]
            Vt_2 = [A.alloc([128, 8, 64], BF16, "Vt%d" % i) for i in range(3)]
        else:
            Vt2 = [al3("Vaug%d" % i, BF16) for i in range(3)]
            for (v_, rv_) in Vt2:
                V("pool", "memset", v_, 1.0, r=[], w=[rv_])
            Wqk32, rW32 = A.alloc([128, 2, 4, 128], F32, "Wqk32")
            P.dma(Wqk32, wqk.rearrange("s h p n -> p s h n"), [], [rW32])
            WQ, rWQ = A.alloc([128, 4, 128], BF16, "WQ")
            WKI, rWKI = A.alloc([128, 4, 256], BF16, "WKI")
            V("dve", "tensor_copy", WQ, Wqk32[:, 0, :, :], r=[rW32], w=[rWQ])
            V("dve", "tensor_scalar", WKI[:, :, 0:128], Wqk32[:, 1, :, :], 128.0 ** -0.5, None, ALU.mult, r=[rW32], w=[rWKI])
            V("dve", "tensor_copy", WKI[:, :, 128:256], bc(identb, 1, 4), r=[rCB], w=[rWKI])
            QT2 = [A.alloc([128, 4, 128], BF16, "QT%d" % i) for i in range(3)]
            KT2 = [A.alloc([128, 4, 128], BF16, "KT%d" % i) for i in range(3)]
            xc_tm2 = [A.alloc([128, 4, 128], BF16, "xc_tm%d" % i) for i in range(3)]
            o_t2 = [A.alloc([128, 512], BF16, "o_t%d" % i) for i in range(3)]
        def dbl(shape, dt_, name):
            return [A.alloc(shape, dt_, name + "_%d" % i) for i in range(3)]

        def dbl3(name, dt_):
            return [al3(name + "_%d" % i, dt_) for i in range(3)]

        K_tm2 = dbl([128, G, 128], BF16, "K_tm")
        cs2 = dbl([128, 16], F32, "cs")
        AU2 = dbl([128, H, 128], BF16, "AU")
        Lp2 = dbl([128, H, 128], BF16, "Lp")
        GTs2 = dbl([128, G, 128], BF16, "GTs")
        MT2 = dbl([128, H, 128], BF16, "MT")
        ea2 = dbl([128, 3, 8], F32, "ea")
        VW2 = dbl3("VW", BF16)
        yt2 = dbl3("yt", F32)
        yprev2 = dbl([128, 512], F32, "yprev")
        yo2 = dbl([128, 512], F32, "yo")
        yb162 = dbl([128, 512], BF16, "yb16")
        zt2 = dbl([128, 512], BF16, "zt")
        zs2 = dbl([128, 512], F32, "zs")
        st12 = dbl([128, 8], F32, "st1")
        junk2_ = dbl([128, 512], F32, "junk")

        seqn = [0]

        def step(tag, c, d_, full, upd, fin):
            tsl = slice(c * 128, (c + 1) * 128)
            pp = seqn[0] % 3
            seqn[0] += 1
            xb, rxb = XB[pp]
            K_tm, rKtm = K_tm2[pp]; cs, rcs = cs2[pp]; AU, rAU = AU2[pp]; Lp, rLp = Lp2[pp]
            GTs, rGTs = GTs2[pp]; MT, rMT = MT2[pp]; ea, rea = ea2[pp]; VW, rVW = VW2[pp]; yt, ryt = yt2[pp]
            yprev, ryp = yprev2[pp]; yo, ryo = yo2[pp]; yb16, ryb = yb162[pp]; zt, rzt = zt2[pp]; zs, rzs = zs2[pp]
            st1, rst1 = st12[pp]; junk, rjunk = junk2_[pp]
            if ssd:
                xs_tm, rxs = xs_tm2[pp]; Vt, rV = Vt_2[pp]
            else:
                QT, rQT = QT2[pp]; KT, rKT = KT2[pp]; xc_tm, rxc = xc_tm2[pp]; o_t, ro = o_t2[pp]
            cvd = convT[(mx, tag)]
            P.dma(xb, cvd.rearrange("j p t -> p j t")[:, :, tsl], ["cvd_%s_%s" % (mx, tag)], [rxb], q="sp")
            S32_, rS32 = S32[d_]
            Sb_, rSb = Sbf[d_]
            if full and fin:
                if ssd:
                    P.dma(yprev, yf_ssd[tsl, :], ["yf_ssd"], [ryp], q="pool")
                    P.dma(zt, z_tm[tsl, :], ["dst"], [rzt], q="pool")
                else:
                    P.dma(yprev, hf_ml[tsl, :], ["hf_ml"], [ryp], q="pool")
                    P.dma(o_t, o_tm[tsl, :], ["dst"], [ro], q="pool")
            if ssd:
                a_col = a_all[tag][0][:, c, d_ * 8:(d_ + 1) * 8]
                ra_col = a_all[tag][1]
                pb, rpb = bank(5)
                for j in range(4):
                    P.mm(pb[:, j * 128:(j + 1) * 128], xb[:, j, :], identb, True, True, [rxb, rCB], rpb)
                dtv = dt_all[tag][0][:, c, d_ * 8:(d_ + 1) * 8]
                V("dve", "tensor_tensor", Vt, pb.rearrange("p (h e) -> p h e", h=8), bc(dtv, 2, 64), ALU.mult, r=rpb + [dt_all[tag][1]], w=[rV])
                if fin:
                    V("act", "copy", xs_tm, pb.rearrange("p (h e) -> p h e", h=8), r=rpb, w=[rxs])
                pb7, rpb7 = bank(7)
                for g in range(2):
                    P.mm(pb7[:, g * 128:(g + 1) * 128], xb[:, 4 + g, :], identb, True, True, [rxb, rCB], rpb7)
                V("act", "copy", K_tm, pb7[:, 0:256].rearrange("p (g n) -> p g n", g=2), r=rpb7, w=[rKtm])
                qT = lambda g: xb[:, 6 + g, :]
                kT = lambda g: xb[:, 4 + g, :]
                rq = [rxb]
                Vv, rVv = Vt, rV
                gi = None
                rgi = None
                p0, rp0 = PS[:, 3, 256:272], ["b3"]
            else:
                a_col = af_all[tag][0][:, c, d_ * 4:(d_ + 1) * 4]
                ra_col = af_all[tag][1]
                gi = gi_all[tag][0][:, c, d_ * 4:(d_ + 1) * 4]
                rgi = gi_all[tag][1]
                Vv, rVv = Vt2[pp]
                P.dma(Vv[:, :, 0:128], v_tm[tag][tsl, :].rearrange("t (h e) -> t h e", h=4), ["dst"], [rVv], q="pool")
                pq, rpq = bank(1)
                pk, rpk = bank(2)
                for h in range(4):
                    P.mm(pq[:, h * 128:(h + 1) * 128], WQ[:, h, :], xb[:, h, :], True, True, [rWQ, rxb], rpq)
                for h in range(4):
                    P.mm(pk[:, h * 128:(h + 1) * 128], WKI[:, h, 0:128], xb[:, h, :], True, True, [rWKI, rxb], rpk)
                V("act", "copy", QT, pq.rearrange("p (h n) -> p h n", h=4), r=rpq, w=[rQT])
                V("dve", "tensor_copy", KT, pk.rearrange("p (h n) -> p h n", h=4), r=rpk, w=[rKT])
                pkx, rpkx = bank(1, 2)
                for h in range(4):
                    P.mm(pkx[:, h // 2, (h % 2) * 256:(h % 2) * 256 + 256], xb[:, h, :], WKI[:, h, :], True, True, [rxb, rWKI], rpkx)
                pkx4 = pkx.rearrange("p b (h n) -> p (b h) n", h=2)
                V("act", "copy", K_tm, pkx4[:, :, 0:128], r=rpkx, w=[rKtm])
                if fin:
                    for b_ in range(2):
                        V("act", "copy", xc_tm[:, 2 * b_:2 * b_ + 2, :], pkx[:, b_, :].rearrange("p (h n) -> p h n", h=2)[:, :, 128:256], r=rpkx, w=[rxc])
                qT = lambda g: QT[:, g, :]
                kT = lambda g: KT[:, g, :]
                rq = [rQT, rKT]
                p0, rp0 = PS[:, 0, 0:16], ["b0"]
            P.mm(p0[:, 0:H], U32[d_], a_col, True, True, [rC32, ra_col], rp0)
            P.mm(p0[:, 8:8 + H], ones32, a_col, True, True, [rC32, ra_col], rp0)
            if ssd:
                V("act", "copy", cs, p0[:, 0:16], r=rp0, w=[rcs])
            else:
                V("dve", "tensor_copy", cs, p0[:, 0:16], r=rp0, w=[rcs])
            if full:
                V("dve", "tensor_tensor", AU, bc(Ub[d_], 1, H), bc(a_col, 2, 128), ALU.mult, r=[rCB, ra_col], w=[rAU])
            if upd:
                V("dve", "tensor_tensor", ea[:, 2, 0:H], cs[:, 8:8 + H], cs[:, 0:H], ALU.subtract, r=[rcs], w=[rea])
                if not ssd:
                    V("dve", "tensor_tensor", ea[:, 2, 0:H], ea[:, 2, 0:H], gi, ALU.add, r=[rea, rgi], w=[rea])
            yield
            if full:
                pR, rpR = bank(1, 2)
                for hb in range(nR):
                    hs = slice(hb * 4, hb * 4 + 4)
                    P.mm(pR[:, hb, :], onesb, AU[:, hs, :].rearrange("p h n -> p (h n)"), True, False, [rCB, rAU], rpR)
                    for hh in range(hb * 4, hb * 4 + 4):
                        P.mm(pR[:, hb, (hh % 4) * 128:(hh % 4) * 128 + 128], AU[:, hh, :], negones, False, False, [rAU, rnegones], rpR)
                    P.mm(pR[:, hb, :], identb, MNrep[d_][0].rearrange("p h n -> p (h n)"), False, True, [rCB, MNrep[d_][1]], rpR)
                if ssd:
                    for hb in range(2):
                        P.act(Lp[:, hb * 4:hb * 4 + 4, :], pR[:, hb, :].rearrange("p (h n) -> p h n", h=4), AF.Exp, rpR, [rLp])
                else:
                    for h in range(4):
                        P.act(Lp[:, h, :], pR[:, 0, h * 128:(h + 1) * 128], AF.Exp, rpR + [rgi], [rLp], bias=gi[:, h:h + 1])
                pG, rpG = bank(3)
                for g in range(G):
                    P.mm(pG[:, g * 128:(g + 1) * 128], kT(g), qT(g), True, True, rq, rpG)
                V("act", "copy", GTs, pG[:, 0:G * 128].rearrange("p (g n) -> p g n", g=G), r=rpG, w=[rGTs])
                if ssd:
                    V("dve", "tensor_tensor", MT.rearrange("p (g j) n -> p g j n", g=2), Lp.rearrange("p (g j) n -> p g j n", g=2), bc(GTs, 2, 4), ALU.mult, r=[rLp, rGTs], w=[rMT])
                else:
                    V("dve", "tensor_tensor", MT, Lp, GTs, ALU.mult, r=[rLp, rGTs], w=[rMT])
                P.act(ea[:, 0, 0:H], cs[:, 0:H], AF.Exp, [rcs], [rea])
            if upd:
                P.act(ea[:, 2, 0:H], ea[:, 2, 0:H], AF.Exp, [rea], [rea])
                P.act(ea[:, 1, 0:H], cs[:, 8:8 + H], AF.Exp, [rcs], [rea])
                V("pool", "tensor_tensor", VW, Vv, bc(ea[:, 2, 0:H], 2, dv), ALU.mult, r=[rVv, rea], w=[rVW])
            yield
            if full:
                pY, rpY = bank(4, nb)
                pO, rpO = bank(6, nb)

                def hv(pt, h):
                    if ssd:
                        return pt[:, h * 64:(h + 1) * 64]
                    return pt[:, h // 2, (h % 2) * 256:(h % 2) * 256 + dv]

                for h in range(H):
                    P.mm(hv(pY, h), MT[:, h, :], Vv[:, h, :], True, True, [rMT, rVv], rpY)
                if ssd:
                    for g in range(2):
                        P.mm(pO[:, g * 256:(g + 1) * 256], qT(g), Sb_[:, g * 4:(g + 1) * 4, :].rearrange("p h e -> p (h e)"), True, True, rq + [rSb], rpO)
                else:
                    for h in range(4):
                        P.mm(hv(pO, h), qT(h), Sb_[:, h, :], True, True, rq + [rSb], rpO)
                if ssd:
                    pY3 = pY.rearrange("p (h e) -> p h e", h=8)
                    pO3 = pO.rearrange("p (h e) -> p h e", h=8)
                else:
                    pY3 = pY.rearrange("p b (h n) -> p (b h) n", h=2)[:, :, 0:dv]
                    pO3 = pO.rearrange("p b (h n) -> p (b h) n", h=2)[:, :, 0:dv]
                V("dve", "tensor_tensor", yt, pO3, bc(ea[:, 0, 0:H], 2, dv), ALU.mult, r=rpO + [rea], w=[ryt])
                V("dve", "tensor_tensor", yt, yt, pY3, ALU.add, r=rpY + [ryt], w=[ryt])
            if upd:
                if ssd:
                    pC, rpC = bank(0)
                    for g in range(2):
                        P.mm(pC[:, g * 256:(g + 1) * 256], K_tm[:, g, :], VW[:, g * 4:(g + 1) * 4, :].rearrange("p h e -> p (h e)"), True, True, [rKtm, rVW], rpC)
                    pC3 = pC.rearrange("p (h e) -> p h e", h=8)
                else:
                    pC, rpC = bank(4, 2)
                    for h in range(4):
                        P.mm(pC[:, h // 2, (h % 2) * 256:(h % 2) * 256 + dv], K_tm[:, h, :], VW[:, h, :], True, True, [rKtm, rVW], rpC)
                    pC3 = pC.rearrange("p b (h n) -> p (b h) n", h=2)[:, :, 0:dv]
                V("dve", "tensor_tensor", S32_, S32_, bc(ea[:, 1, 0:H], 2, dv), ALU.mult, r=[rS32, rea], w=[rS32])
                V("dve", "tensor_tensor", S32_, S32_, pC3, ALU.add, r=[rS32] + rpC, w=[rS32])
                V("act", "copy", Sb_, S32_, r=[rS32], w=[rSb])
            if full:
                if ssd:
                    yflat = yt.rearrange("p h e -> p (h e)")
                    if not fin:
                        P.dma(yf_ssd[tsl, :], yflat, [ryt], ["yf_ssd"], q="pool")
                    else:
                        V("dve", "tensor_tensor", yo, yflat, yprev, ALU.add, r=[ryt, ryp], w=[ryo])
                        V("pool", "tensor_tensor", yprev.rearrange("p (h e) -> p h e", h=8), xs_tm, bc(Dh_bc, 2, 64), ALU.mult, r=[rxs, rssdrow], w=[ryp])
                        V("dve", "tensor_tensor", yo, yo, yprev, ALU.add, r=[ryo, ryp], w=[ryo])
                        P.act(zs, zt, AF.Silu, [rzt], [rzs])
                        V("dve", "tensor_tensor", yo, yo, zs, ALU.mult, r=[ryo, rzs], w=[ryo])
                        P.act(junk, yo, AF.Square, [ryo], [rjunk, rst1], accum_out=st1[:, 0:1])
                        P.act(st1[:, 1:2], st1[:, 0:1], AF.Sqrt, [rst1], [rst1], scale=1.0 / 512, bias=EPS)
                        V("dve", "reciprocal", st1[:, 2:3], st1[:, 1:2], r=[rst1], w=[rst1])
                        V("dve", "scalar_tensor_tensor", yb16, yo, st1[:, 2:3], ssdnw_bc, ALU.mult, ALU.mult, r=[ryo, rst1, rssdrow], w=[ryb])
                        P.dma(ymix[tsl, 0:512], yb16, [ryb], ["ymix"], q="sp")
                else:
                    P.act(st1[:, 0:4], yt[:, :, 128], AF.Abs, [ryt], [rst1])
                    V("dve", "tensor_scalar", st1[:, 0:4], st1[:, 0:4], 1.0, None, ALU.max, r=[rst1], w=[rst1])
                    V("dve", "reciprocal", st1[:, 4:8], st1[:, 0:4], r=[rst1], w=[rst1])
                    yo3 = yo.rearrange("p (h e) -> p h e", h=4)
                    V("dve", "tensor_tensor", yo3, yt[:, :, 0:128], bc(st1[:, 4:8], 2, 128), ALU.mult, r=[ryt, rst1], w=[ryo])
                    if not fin:
                        P.dma(hf_ml[tsl, :], yo, [ryo], ["hf_ml"], q="pool")
                    else:
                        V("dve", "tensor_tensor", yo, yo, yprev, ALU.add, r=[ryo, ryp], w=[ryo])
                        P.act(zs, o_t, AF.Tanh, [ro], [rzs], scale=0.5)
                        V("dve", "scalar_tensor_tensor", yo, zs, 1.0, yo, ALU.add, ALU.mult, r=[ryo, rzs], w=[ryo])
                        for h in range(4):
                            P.act(junk[:, h * 128:(h + 1) * 128], yo[:, h * 128:(h + 1) * 128], AF.Square, [ryo], [rjunk, rst1], accum_out=st1[:, h:h + 1])
                        P.act(st1[:, 0:4], st1[:, 0:4], AF.Sqrt, [rst1], [rst1], scale=1.0 / 128, bias=4.0 * EPS)
                        V("dve", "reciprocal", st1[:, 4:8], st1[:, 0:4], r=[rst1], w=[rst1])
                        V("dve", "tensor_tensor", yo3, yo3, bc(st1[:, 4:8], 2, 128), ALU.mult, r=[ryo, rst1], w=[ryo])
                        V("dve", "tensor_tensor", yo, yo, mlnw_bc, ALU.mult, r=[ryo, rmlrow], w=[ryo])
                        V("pool", "tensor_tensor", yprev, xc_tm.rearrange("p h e -> p (h e)"), mlskip_bc, ALU.mult, r=[rxc, rmlrow], w=[ryp])
                        V("dve", "tensor_tensor", yb16, yo, yprev, ALU.add, r=[ryo, ryp], w=[ryb])
                        ymv = ymix.rearrange("(r c) d -> r c d", c=64)
                        P.dma(ymv[:, 2 * c, 512:1024], yb16[0:32, :], [ryb], ["ymix"], q="sp")
                        P.dma(ymv[:, 2 * c + 1, 512:1024], yb16[64:96, :], [ryb], ["ymix"], q="sp")

        def run_pipelined(specs):
            st_a1 = None
            st_a2 = None
            for sp_ in list(specs) + [None, None]:
                g = None
                if sp_ is not None:
                    g = step(*sp_)
                    next(g)
                if st_a1 is not None:
                    next(st_a1)
                if st_a2 is not None:
                    for _ in st_a2:
                        pass
                st_a2 = st_a1
                st_a1 = g

        for d_ in range(2):
            V("pool", "memset", S32[d_][0], 0.0, r=[], w=[S32[d_][1]])
            V("pool", "memset", Sbf[d_][0], 0.0, r=[], w=[Sbf[d_][1]])
        h1 = [("ctx", 0, 0, False, True, False), ("ctx", 1, 1, False, True, False),
              ("ctx", 1, 0, False, True, False), ("ctx", 0, 1, False, True, False)]
        if ssd:
            for i in range(16):
                h1.append(("lat", i, 0, True, i < 15, False))
                h1.append(("lat", 31 - i, 1, False, True, False))
            h2 = [("lat", c, 1, True, c > 0, True) for c in range(15, -1, -1)]
        else:
            for i in range(16):
                h1.append(("lat", i, 0, True, True, False))
                h1.append(("lat", 31 - i, 1, True, True, False))
            h2 = []
            for i in range(16, 32):
                h2.append(("lat", i, 0, True, i < 31, True))
                h2.append(("lat", 31 - i, 1, True, 31 - i > 0, True))
        run_pipelined(h1)
        run_pipelined(h2)
        P.fence()
        A.release(m1)

    def mod_rows():
        g = {}
        modr, rmodr = A.alloc([128, 4, 1024], F32, "modr")
        fnw, rfnw = bcrow(fnw_row, 1024, "fnw", q="pool")
        mD = A.mark()
        for v in range(4):
            P.dma(modr[:, v, :], modrow[:, v * D:(v + 1) * D].partition_broadcast(128), ["modrow"], [rmodr], q=("sp" if v % 2 == 0 else "act"))
        g1_bc, sh2_bc, g2_bc = modr[:, 0, :], modr[:, 1, :], modr[:, 3, :]
        n2w, rn2w = bcrow(n2w_row, 1024, "n2w")
        s2_bc = modr[:, 2, :]
        V("dve", "scalar_tensor_tensor", s2_bc, s2_bc, 1.0, n2w, ALU.add, ALU.mult, r=[rmodr, rn2w], w=[rmodr])

        return dict(modr=modr, rmodr=rmodr, fnw=fnw, rfnw=rfnw, mD=mD, g1_bc=g1_bc, sh2_bc=sh2_bc, g2_bc=g2_bc, s2_bc=s2_bc)

    import os
    STOP = int(os.environ.get("KSTOP", "99"))
    if STOP >= 1:
        inproj_pass("ssd")
    if STOP >= 2:
        scan_mixer("ssd")
    if STOP >= 3:
        inproj_pass("ml")
    MR = mod_rows()
    if STOP >= 4:
        scan_mixer("ml")
    if STOP < 5:
        P.finalize(st)
        st.close()
        print("sem counts", P.sem_counts, "nops", len(P.ops), "sim_us", P.sim_time)
        return nc

    modr, rmodr, fnw, rfnw = MR["modr"], MR["rmodr"], MR["fnw"], MR["rfnw"]
    g1_bc, sh2_bc, g2_bc, s2_bc = MR["g1_bc"], MR["sh2_bc"], MR["g2_bc"], MR["s2_bc"]
    A.release(MR["mD"])
    tT, rtT = A.alloc([128, 8, TOWN], BF16, "tT")
    comb, rcomb = A.alloc([128, 16, 32], F32, "comb")
    xt, rxt = A.alloc([128, 1024], F32, "xt")
    x1, rx1 = A.alloc([128, 1024], F32, "x1")
    junk2, rjunk2 = A.alloc([128, 1024], F32, "junk2")
    s8, rs8 = A.alloc([128, 8], F32, "s8")
    mD = A.mark()
    mstg = [A.alloc([128, 8, 512], F32, "mstgD%d" % i) for i in range(2)]
    rbb, rrbb = bcrow(rb_row, 36, "rbb")
    Wo, rWo = A.alloc([128, 8, 1024], BF16, "Wo")
    wout_v = w_out.rearrange("(k p) n -> p k n", p=128)
    for hb in range(2):
        ms, rms = mstg[hb]
        P.dma(ms, wout_v[:, :, hb * 512:(hb + 1) * 512], [], [rms], q=("sp" if hb == 0 else "pool"))
        V("dve" if hb == 0 else "pool", "tensor_tensor", Wo[:, :, hb * 512:(hb + 1) * 512], ms,
          bc(g1_bc[:, hb * 512:(hb + 1) * 512], 1, 8), ALU.mult, r=[rms, rmodr], w=[rWo])
    Wr32, rWr32 = A.alloc([128, 8, 36], F32, "Wr32")
    Wr, rWr = A.alloc([128, 8, 36], BF16, "Wr")
    P.dma(Wr32, w_r.rearrange("(k p) n -> p k n", p=128), [], [rWr32])
    V("dve", "tensor_copy", Wr, Wr32, r=[rWr32], w=[rWr])
    ym2 = [A.alloc([128, 1024], BF16, "ym%d" % i) for i in range(2)]
    yT2 = [A.alloc([128, 8, 128], BF16, "yT%d" % i) for i in range(2)]
    tb162 = [A.alloc([128, 1024], BF16, "tb16%d" % i) for i in range(2)]
    lg_all, rlg_all = A.alloc([128, 16, 36], F32, "lg_all")
    xtD = [A.alloc([128, 1024], F32, "xtD%d" % i) for i in range(2)]
    x1D = [A.alloc([128, 1024], F32, "x1D%d" % i) for i in range(2)]
    jkD = [A.alloc([128, 1024], F32, "jkD%d" % i) for i in range(2)]
    s8D = [A.alloc([128, 8], F32, "s8D%d" % i) for i in range(2)]
    xt_f, rxt_f, x1_f, rx1_f, junk2_f, rjunk2_f, s8_f, rs8_f = xt, rxt, x1, rx1, junk2, rjunk2, s8, rs8
    for i in range(16):
        tsl = slice(i * 128, (i + 1) * 128)
        ym, rym = ym2[i % 2]; yT, ryT = yT2[i % 2]; tb16, rtb16 = tb162[i % 2]
        xt, rxt = xtD[i % 2]; x1, rx1 = x1D[i % 2]; junk2, rjunk2 = jkD[i % 2]; s8, rs8 = s8D[i % 2]
        P.dma(ym, ymix[tsl, :], ["ymix"], [rym], q="sp")
        P.dma(xt, x_own[tsl, :], [], [rxt], q="pool")
        pT, rpT = bank(0, 2)
        for k in range(8):
            P.mm(pT[:, k // 4, (k % 4) * 128:(k % 4) * 128 + 128], ym[:, k * 128:(k + 1) * 128], identb, True, True, [rym, rCB], rpT)
        V("act", "copy", yT[:, 0:4, :], pT[:, 0, :].rearrange("p (k n) -> p k n", k=4), r=rpT, w=[ryT])
        V("act", "copy", yT[:, 4:8, :], pT[:, 1, :].rearrange("p (k n) -> p k n", k=4), r=rpT, w=[ryT])
        pX, rpX = bank(2, 2)
        for hb in range(2):
            for k in range(8):
                P.mm(pX[:, hb, :], yT[:, k, :], Wo[:, k, hb * 512:(hb + 1) * 512], k == 0, k == 7, [ryT, rWo], rpX)
        V("dve", "tensor_tensor", x1, pX.rearrange("p b n -> p (b n)"), xt, ALU.add, r=rpX + [rxt], w=[rx1])
        P.dma(x1s[tsl, :], x1, [rx1], ["x1s"], q="pool")
        P.act(junk2, x1, AF.Square, [rx1], [rjunk2, rs8], accum_out=s8[:, 0:1])
        P.act(s8[:, 1:2], s8[:, 0:1], AF.Sqrt, [rs8], [rs8], scale=1.0 / D, bias=EPS)
        V("dve", "reciprocal", s8[:, 2:3], s8[:, 1:2], r=[rs8], w=[rs8])
        V("dve", "scalar_tensor_tensor", junk2, x1, s8[:, 2:3], s2_bc, ALU.mult, ALU.mult, r=[rx1, rs8, rmodr], w=[rjunk2])
        V("dve", "tensor_tensor", tb16, junk2, sh2_bc, ALU.add, r=[rjunk2, rmodr], w=[rtb16])
        pT2, rpT2 = bank(4, 2)
        for k in range(8):
            P.mm(pT2[:, k // 4, (k % 4) * 128:(k % 4) * 128 + 128], tb16[:, k * 128:(k + 1) * 128], identb, True, True, [rtb16, rCB], rpT2)
        V("act", "copy", tT[:, 0:4, tsl], pT2[:, 0, :].rearrange("p (k n) -> p k n", k=4), r=rpT2, w=[rtT])
        V("act", "copy", tT[:, 4:8, tsl], pT2[:, 1, :].rearrange("p (k n) -> p k n", k=4), r=rpT2, w=[rtT])
        pL, rpL = bank(6)
        for k in range(8):
            P.mm(pL[:, 0:36], tT[:, k, tsl], Wr[:, k, :], k == 0, k == 7, [rtT, rWr], rpL)
        V("dve", "tensor_tensor", lg_all[:, i, :], pL[:, 0:36], rbb, ALU.add, r=rpL + [rrbb], w=[rlg_all])

    AXX = mybir.AxisListType.X
    NT = 16
    gl = lg_all[:, :, 0:4]
    el = lg_all[:, :, 4:36].rearrange("p t (g e) -> p t g e", g=4)

    def ra(shape, name):
        return A.alloc(shape, F32, name)

    gmax, rgmax = ra([128, NT], "gmax")
    ohg, rohg = ra([128, NT, 4], "ohg")
    gd, rgd = ra([128, NT, 4], "gd")
    pg, rpg_ = ra([128, NT], "pg")
    prod, rprod = ra([128, NT, 4, 8], "prod")
    esel, resel = ra([128, NT, 8], "esel")
    m1, rm1 = ra([128, NT], "m1")
    m2, rm2 = ra([128, NT], "m2")
    oh1, roh1 = ra([128, NT, 8], "oh1")
    msk, rmsk = ra([128, NT, 8], "msk")
    oh2, roh2 = ra([128, NT, 8], "oh2")
    w1, rw1 = ra([128, NT], "w1")
    w2, rw2 = ra([128, NT], "w2")
    wig, rwig = ra([128, NT, 8], "wig")
    V("dve", "reduce_max", gmax, gl, AXX, r=[rlg_all], w=[rgmax])
    V("dve", "tensor_tensor", ohg, gl, bc(gmax, 2, 4), ALU.is_equal, r=[rlg_all, rgmax], w=[rohg])
    V("dve", "tensor_tensor", gd, gl, bc(gmax, 2, 4), ALU.subtract, r=[rlg_all, rgmax], w=[rgd])
    P.act(gd, gd, AF.Exp, [rgd], [rgd])
    V("dve", "reduce_sum", pg, gd, AXX, r=[rgd], w=[rpg_])
    V("dve", "reciprocal", pg, pg, r=[rpg_], w=[rpg_])
    V("dve", "tensor_tensor", prod, el, bc(ohg, 3, 8), ALU.mult, r=[rlg_all, rohg], w=[rprod])
    V("dve", "reduce_sum", esel, prod.rearrange("p t g e -> p t e g"), AXX, r=[rprod], w=[resel])
    V("dve", "reduce_max", m1, esel, AXX, r=[resel], w=[rm1])
    V("dve", "tensor_tensor", oh1, esel, bc(m1, 2, 8), ALU.is_equal, r=[resel, rm1], w=[roh1])
    V("dve", "scalar_tensor_tensor", msk, oh1, -1e30, esel, ALU.mult, ALU.add, r=[roh1, resel], w=[rmsk])
    V("dve", "reduce_max", m2, msk, AXX, r=[rmsk], w=[rm2])
    V("dve", "tensor_tensor", oh2, msk, bc(m2, 2, 8), ALU.is_equal, r=[rmsk, rm2], w=[roh2])
    V("dve", "tensor_tensor", w1, m1, m2, ALU.subtract, r=[rm1, rm2], w=[rw1])
    P.act(w1, w1, AF.Sigmoid, [rw1], [rw1])
    V("dve", "tensor_tensor", w1, w1, pg, ALU.mult, r=[rw1, rpg_], w=[rw1])
    V("dve", "tensor_tensor", w2, pg, w1, ALU.subtract, r=[rpg_, rw1], w=[rw2])
    V("dve", "tensor_tensor", wig, oh1, bc(w1, 2, 8), ALU.mult, r=[roh1, rw1], w=[rwig])
    V("dve", "tensor_tensor", oh2, oh2, bc(w2, 2, 8), ALU.mult, r=[roh2, rw2], w=[roh2])
    V("dve", "tensor_tensor", wig, wig, oh2, ALU.add, r=[rwig, roh2], w=[rwig])
    V("dve", "tensor_tensor", comb.rearrange("p t (g e) -> p t g e", g=4), bc(ohg, 3, 8), bc(wig, 2, 4), ALU.mult, r=[rohg, rwig], w=[rcomb])

    xt, rxt, x1, rx1, junk2, rjunk2, s8, rs8 = xt_f, rxt_f, x1_f, rx1_f, junk2_f, rjunk2_f, s8_f, rs8_f
    P.fence()
    A.release(mD)
    acc, racc = A.alloc([128, 16, 1024], F32, "acc")
    for i in range(16):
        P.dma(acc[:, i, :], x1s[i * 128:(i + 1) * 128, :], ["x1s"], [racc + "_%d" % i], q="act")
    mE = A.mark()
    wst32 = [A.alloc([128, 8, 256], F32, "wst32_%d" % i) for i in range(3)]
    Wg_ = [A.alloc([128, 8, 256], BF16, "Wg%d" % i) for i in range(2)]
    Wu_ = [A.alloc([128, 8, 256], BF16, "Wu%d" % i) for i in range(2)]
    Wd_ = [A.alloc([128, 2, 1024], BF16, "Wd%d" % i) for i in range(2)]
    sg = [[A.alloc([128, 512], BF16, "sg%d_%d" % (i, j)) for j in range(2)] for i in range(2)]
    hTt = [[A.alloc([128, 512], BF16, "hT%d_%d" % (i, j)) for j in range(2)] for i in range(2)]
    Wcur = {}

    def moe_gu(e, tb, par):
        if tb == 0:
            Wg, rWg = Wg_[e % 2]
            Wu, rWu = Wu_[e % 2]
            Wd, rWd = Wd_[e % 2]
            s0, rs0 = wst32[0]
            s1_, rs1_ = wst32[1]
            s2_, rs2_ = wst32[2]
            P.dma(s0, wg[e].rearrange("(k p) f -> p k f", p=128), [], [rs0], q="sp")
            V("pool", "tensor_copy", Wg, s0, r=[rs0], w=[rWg])
            P.dma(s1_, wu[e].rearrange("(k p) f -> p k f", p=128), [], [rs1_], q="sp")
            V("pool", "tensor_copy", Wu, s1_, r=[rs1_], w=[rWu])
            s2v = s2_.rearrange("p k f -> p (k f)").rearrange("p (c n) -> p c n", c=2)
            P.dma(s2v, wd[e].rearrange("(c p) n -> p c n", p=128), [], [rs2_], q="sp")
            V("pool", "tensor_tensor", Wd, s2v, bc(g2_bc, 1, 2), ALU.mult, r=[rs2_, rmodr], w=[rWd])
        Wg, rWg = Wg_[e % 2]
        Wu, rWu = Wu_[e % 2]
        tbs = slice(tb * 512, (tb + 1) * 512)
        for fh in range(2):
            pg, rpg = bank(fh)
            pu, rpu = bank(2 + fh)
            for k in range(8):
                P.mm(pg, Wg[:, k, fh * 128:(fh + 1) * 128], tT[:, k, tbs], k == 0, k == 7, [rWg, rtT], rpg)
            for k in range(8):
                P.mm(pu, Wu[:, k, fh * 128:(fh + 1) * 128], tT[:, k, tbs], k == 0, k == 7, [rWu, rtT], rpu)
            P.act(sg[fh][par][0], pg, AF.Silu, rpg, [sg[fh][par][1]])
            V("dve", "tensor_tensor", hTt[fh][par][0], sg[fh][par][0], pu, ALU.mult, r=[sg[fh][par][1]] + rpu, w=[hTt[fh][par][1]])

    def moe_down(e, tb, par):
        Wd, rWd = Wd_[e % 2]
        for st_ in range(4):
            ti = tb * 4 + st_
            po, rpo = bank(4 + 2 * (st_ % 2), 2)
            for dh in range(2):
                for fc in range(2):
                    P.mm(po[:, dh, :], hTt[fc][par][0][:, st_ * 128:(st_ + 1) * 128], Wd[:, fc, dh * 512:(dh + 1) * 512], fc == 0, fc == 1, [hTt[fc][par][1], rWd], rpo)
            ra_ = racc + "_%d" % ti
            V("dve", "scalar_tensor_tensor", acc[:, ti, :], po.rearrange("p b n -> p (b n)"), comb[:, ti, e:e + 1], acc[:, ti, :], ALU.mult, ALU.add, r=rpo + [rcomb, ra_], w=[ra_])

    blks = [(e, tb) for e in range(32) for tb in range(4)]
    moe_gu(blks[0][0], blks[0][1], 0)
    for i_, (e, tb) in enumerate(blks):
        if i_ + 1 < len(blks):
            moe_gu(blks[i_ + 1][0], blks[i_ + 1][1], (i_ + 1) % 2)
        moe_down(e, tb, i_ % 2)

    P.fence()
    A.release(mE)
    jkF = [A.alloc([128, 1024], F32, "jkF%d" % i) for i in range(2)]
    s8F = [A.alloc([128, 8], F32, "s8F%d" % i) for i in range(2)]
    for i in range(16):
        tsl = slice(i * 128, (i + 1) * 128)
        junk2, rjunk2 = jkF[i % 2]; s8, rs8 = s8F[i % 2]
        x2, rx2 = acc[:, i, :], racc + "_%d" % i
        P.act(junk2, x2, AF.Square, [rx2], [rjunk2, rs8], accum_out=s8[:, 0:1])
        P.act(s8[:, 1:2], s8[:, 0:1], AF.Sqrt, [rs8], [rs8], scale=1.0 / D, bias=EPS)
        V("dve", "reciprocal", s8[:, 2:3], s8[:, 1:2], r=[rs8], w=[rs8])
        V("dve", "scalar_tensor_tensor", junk2, x2, s8[:, 2:3], fnw, ALU.mult, ALU.mult, r=[rx2, rs8, rfnw], w=[rjunk2])
        P.dma(out[tsl, :], junk2, [rjunk2], ["out"], q=("sp" if i % 2 == 0 else "act"))

    P.finalize(st)
    st.close()
    print("sem counts", P.sem_counts, "nops", len(P.ops), "sim_us", P.sim_time)
    return nc


_NC_CACHE = {}


def _host_inputs(inp):
    f = lambda a: np.ascontiguousarray(np.asarray(a, dtype=np.float32))
    x = f(inp["x"]); c = f(inp["c"]); ctx = f(inp["ctx"]); c_ctx = f(inp["c_ctx"])
    L = 0
    w_mod = f(inp["w_mod"][L]); b_mod = f(inp["b_mod"][L]); n1w = f(inp["norm1_w"][L]); w_in = f(inp["w_in"][L])
    eye = np.eye(128, dtype=np.float32)
    k = np.arange(128)
    Uf = (k[:, None] <= k[None, :]).astype(np.float32)
    Ub = (k[:, None] >= k[None, :]).astype(np.float32)
    MNf = np.where(k[None, :] >= k[:, None], 0.0, -30000.0).astype(np.float32)
    MNb = np.where(k[None, :] <= k[:, None], 0.0, -30000.0).astype(np.float32)
    cst = f(np.stack([eye, Uf, Ub, np.ones((128, 128), np.float32), MNf, MNb], axis=1))
    col8 = lambda v: f(v.reshape(8, 128).T)
    bmod_col = f(np.concatenate([col8(b_mod[0:1024]), col8(b_mod[1024:2048])], axis=1))
    w_r = f(np.concatenate([inp["moe_rg_w"][L], inp["moe_re_w"][L]], axis=1))
    rb_row = f(np.concatenate([inp["moe_rg_b"][L], inp["moe_re_b"][L]])[None, :])
    wqk_in = f(inp["ml_w_qk"][L])
    wqk = np.zeros((2, 4, 128, 128), np.float32)
    for s in range(2):
        for n in range(128):
            ch, o = divmod(n * 4, 128)
            wqk[s, ch, o:o + 4, o:o + 4] = wqk_in[s, n]
    shared = dict(w_mod=w_mod, bmod_col=bmod_col, bmod_row=f(b_mod[None, :]), n1w_col=col8(n1w),
                  ssd_cb=f(inp["ssd_conv_b"][L].reshape(8, 128).T), ml_cb=f(inp["ml_conv_b"][L].reshape(4, 128).T),
                  wqk=wqk, w_out=f(inp["w_out"][L]), n2w_row=f(inp["norm2_w"][L][None, :]), w_r=w_r, rb_row=rb_row,
                  wg=f(inp["moe_w_gate"][L]), wu=f(inp["moe_w_up"][L]), wd=f(inp["moe_w_down"][L]),
                  fnw_row=f(inp["final_norm_w"][None, :]), cst=cst)
    maps = []
    for core in range(8):
        b, h = divmod(core, 2)
        rev = h == 1
        xb = x[b]
        cb = ctx[b]
        if rev:
            xb = xb[::-1]
            cb = cb[::-1]
        def blk(a_t):
            n_ = a_t.shape[1]
            return f(a_t.reshape(8, 128, n_ // 256, 256).transpose(2, 1, 0, 3).reshape(n_ // 256, 128, 8 * 256))
        xT_r = blk(xb.T)
        xT_c = blk(xb.reshape(64, 64, D).transpose(1, 0, 2).reshape(T, D).T)
        ctxT = blk(cb.T)
        x_own = f(xb[:TOWN])
        ccol = f(np.stack([c[b].reshape(8, 128).T, c_ctx.reshape(8, 128).T], axis=2))
        wi = w_in.copy()
        scw = f(inp["ssd_conv_w"][L]); mcw = f(inp["ml_conv_w"][L])
        dtb = f(inp["ssd_dt_bias"][L]); alog = f(inp["ssd_a_log"][L]); gb = f(inp["ml_gate_b"][L])
        if rev:
            wi[:, 1536:1552] = np.concatenate([w_in[:, 1544:1552], w_in[:, 1536:1544]], axis=1)
            g0 = 1552 + 1536
            gcols = w_in[:, g0:g0 + 16].reshape(D, 2, 2, 4)[:, :, ::-1, :].reshape(D, 16)
            wi[:, g0:g0 + 16] = gcols
            scw = scw[::-1]; mcw = mcw[::-1]
            dtb = dtb[::-1]; alog = alog[::-1]; gb = gb[:, ::-1, :]
        ssd_cw = f(scw.T.reshape(8, 128, 5).transpose(1, 0, 2))
        ml_cw = f(mcw.T.reshape(4, 128, 5).transpose(1, 0, 2))
        ssd_row = f(np.concatenate([dtb.reshape(-1), alog.reshape(-1), inp["ssd_d"][L], inp["ssd_norm_w"][L]])[None, :])
        ml_row = f(np.concatenate([gb.reshape(-1), inp["ml_norm_w"][L], inp["ml_skip"][L]])[None, :])
        m = dict(shared)
        m.update(xT_r=xT_r, xT_c=xT_c, ctxT=ctxT, x_own=x_own, ccol=ccol, w_in=f(wi), ssd_cw=ssd_cw, ml_cw=ml_cw,
                 ssd_row=ssd_row, ml_row=ml_row)
        maps.append(m)
    return maps


def kernel(**inputs):
    maps = _host_inputs(inputs)
    if "nc" not in _NC_CACHE:
        _NC_CACHE["nc"] = build_nc()
    nc = _NC_CACHE["nc"]
    res = run_bass_kernel_spmd(nc, maps, core_ids=list(range(8)))
    outp = np.zeros((4, T, D), np.float32)
    for core in range(8):
        b, h = divmod(core, 2)
        o = np.asarray(res.results[core]["out"], dtype=np.float32)
        if h == 0:
            outp[b, :TOWN] = o
        else:
            outp[b, TOWN:] = o[::-1]
    kernel.last_results = res
    return outp
```
